# Optimizing a Trainium2 kernel written in Bass

```python
import math
import jax
import jax.numpy as jnp
from jax import lax
import numpy as np

D_MODEL = 1024
BATCH = 32
SEQ = 2048
DEPTH = 1

HEAD_DIM = 64
A_HEADS = 8
A_KV = 2
A_REP = A_HEADS // A_KV
A_WINDOW = 128
B_HEADS = 8
B_KV = 2
B_REP = B_HEADS // B_KV
CMP_LEN = 32
CMP_STRIDE = 16
CMP_HIDDEN = 128
SLC_LEN = 64
SLC_TOP = 8
SLC_LOCAL = 2
B_WINDOW = 512
N_BUCKETS = 32
REL_MAX_DIST = 128
N_EXPERTS = 32
TOP_K = 4
D_FF = D_MODEL
SWIGLU_LIMIT = 7.0
SWIGLU_ALPHA = 1.702
Q_BLOCK = 128
SLC_Q_BLOCK = 64
MOE_BLOCK = 256
LN_EPS = 1e-5
NEG_INF = -1e30
FORCED_SCORE = 1e30
DEEPNORM_ALPHA = (2 * DEPTH) ** 0.25
DEEPNORM_BETA = (8 * DEPTH) ** -0.25
A_WIDTH = A_HEADS * HEAD_DIM
B_WIDTH = B_HEADS * HEAD_DIM
A_KV_WIDTH = A_KV * HEAD_DIM
B_KV_WIDTH = B_KV * HEAD_DIM
IN_WIDTHS = (A_WIDTH, A_KV_WIDTH, A_KV_WIDTH, B_WIDTH, B_KV_WIDTH, B_KV_WIDTH, B_KV_WIDTH, B_KV_WIDTH, B_KV_WIDTH, B_KV_WIDTH, 3 * B_HEADS, 2 * D_MODEL)
IN_WIDTH = sum(IN_WIDTHS)

kernel_name = 'hybrid_swa_nsa_moe_block'


def layer_norm(x, g, b):
    xf = x.astype(jnp.float32)
    mu = jnp.mean(xf, axis=-1, keepdims=True)
    var = jnp.mean(jnp.square(xf - mu), axis=-1, keepdims=True)
    return ((xf - mu) * lax.rsqrt(var + LN_EPS)).astype(x.dtype) * g + b


def t5_bucket(rel):
    n = jnp.maximum(rel, 0)
    max_exact = N_BUCKETS // 2
    nf = jnp.maximum(n, 1).astype(jnp.float32)
    large = max_exact + (jnp.log(nf / max_exact) / math.log(REL_MAX_DIST / max_exact) * (N_BUCKETS - max_exact)).astype(jnp.int32)
    large = jnp.minimum(large, N_BUCKETS - 1)
    return jnp.where(n < max_exact, n, large)


def banded_attention(q, k, v, bias_table, window, sinks=None):
    bsz, seq, g, r, d = q.shape
    n_prev = -(-window // Q_BLOCK)
    kb = (n_prev + 1) * Q_BLOCK
    nb = seq // Q_BLOCK
    pad = ((0, 0), (n_prev * Q_BLOCK, 0), (0, 0), (0, 0))
    kp = jnp.pad(k, pad)
    vp = jnp.pad(v, pad)
    q_off = np.arange(Q_BLOCK)[:, None]
    k_off = np.arange(kb)[None, :]
    rel = n_prev * Q_BLOCK + q_off - k_off
    band = (rel >= 0) & (rel < window)
    bias = jnp.transpose(bias_table[t5_bucket(jnp.asarray(rel))], (2, 3, 0, 1))
    scale = d ** -0.5

    def one_block(i):
        qb = lax.dynamic_slice_in_dim(q, i * Q_BLOCK, Q_BLOCK, axis=1)
        kblk = lax.dynamic_slice_in_dim(kp, i * Q_BLOCK, kb, axis=1)
        vblk = lax.dynamic_slice_in_dim(vp, i * Q_BLOCK, kb, axis=1)
        s = jnp.einsum('bqgrd,bkgd->bgrqk', qb, kblk).astype(jnp.float32) * scale + bias
        kpos = i * Q_BLOCK - n_prev * Q_BLOCK + jnp.asarray(k_off)
        s = jnp.where(band & (kpos >= 0), s, NEG_INF)
        if sinks is None:
            p = jax.nn.softmax(s, axis=-1)
        else:
            sk = sinks.astype(jnp.float32)[None, :, :, None, None]
            m = jnp.maximum(jnp.max(s, axis=-1, keepdims=True), sk)
            e = jnp.exp(s - m)
            p = e / (jnp.sum(e, axis=-1, keepdims=True) + jnp.exp(sk - m))
        return jnp.einsum('bgrqk,bkgd->bqgrd', p.astype(v.dtype), vblk)

    out = lax.map(one_block, jnp.arange(nb))
    return jnp.moveaxis(out, 0, 1).reshape(bsz, seq, g, r, d)


def compress_blocks(kv, pos, w1, w2):
    bsz, seq, g, d = kv.shape
    nc = (seq - CMP_LEN) // CMP_STRIDE + 1
    idx = np.arange(nc)[:, None] * CMP_STRIDE + np.arange(CMP_LEN)[None, :]
    blocks = kv[:, idx] + pos[None, None, :, None, :]
    blocks = jnp.transpose(blocks, (0, 1, 3, 2, 4)).reshape(bsz, nc, g, CMP_LEN * d)
    return jax.nn.gelu(blocks @ w1) @ w2


def cmp_to_slc_matrix(nc, nslc):
    cs = np.arange(nc)[:, None] * CMP_STRIDE
    ss = np.arange(nslc)[None, :] * SLC_LEN
    ov = np.clip(np.minimum(cs + CMP_LEN, ss + SLC_LEN) - np.maximum(cs, ss), 0, None)
    return (ov / CMP_LEN).astype(np.float32)


def selected_attention(q, k, v, sel, bias_table):
    bsz, seq, g, r, d = q.shape
    nslc = seq // SLC_LEN
    top = sel.shape[-1]
    kt = jnp.transpose(k.reshape(bsz, nslc, SLC_LEN, g, d), (0, 3, 1, 2, 4))
    vt = jnp.transpose(v.reshape(bsz, nslc, SLC_LEN, g, d), (0, 3, 1, 2, 4))
    b_idx = jnp.arange(bsz)[:, None, None, None]
    g_idx = jnp.arange(g)[None, None, :, None]
    g_idx5 = jnp.arange(g)[None, None, :, None, None]
    scale = d ** -0.5

    def one_chunk(i):
        qc = lax.dynamic_slice_in_dim(q, i * SLC_Q_BLOCK, SLC_Q_BLOCK, axis=1)
        sc = lax.dynamic_slice_in_dim(sel, i * SLC_Q_BLOCK, SLC_Q_BLOCK, axis=1)
        kg = kt[b_idx, g_idx, sc]
        vg = vt[b_idx, g_idx, sc]
        kpos = sc[..., None] * SLC_LEN + jnp.arange(SLC_LEN)
        tpos = i * SLC_Q_BLOCK + jnp.arange(SLC_Q_BLOCK)
        rel = tpos[None, :, None, None, None] - kpos
        bias = jnp.moveaxis(bias_table[t5_bucket(rel), g_idx5], -1, 3)
        s = jnp.einsum('bqgrd,bqgnld->bqgrnl', qc, kg).astype(jnp.float32) * scale + bias
        s = jnp.where((rel >= 0)[:, :, :, None], s, NEG_INF)
        p = jax.nn.softmax(s.reshape(s.shape[:4] + (top * SLC_LEN,)), axis=-1).reshape(s.shape)
        return jnp.einsum('bqgrnl,bqgnld->bqgrd', p.astype(v.dtype), vg)

    out = lax.map(one_chunk, jnp.arange(seq // SLC_Q_BLOCK))
    return jnp.moveaxis(out, 0, 1).reshape(bsz, seq, g, r, d)


def nsa_attention(q, k_cmp, v_cmp, k_slc, v_slc, k_win, v_win, gate_logits, bias_table,
                  cmp_pos_k, cmp_w1_k, cmp_w2_k, cmp_pos_v, cmp_w1_v, cmp_w2_v):
    bsz, seq, g, r, d = q.shape
    scale = d ** -0.5
    t = np.arange(seq)
    kc = compress_blocks(k_cmp, cmp_pos_k, cmp_w1_k, cmp_w2_k)
    vc = compress_blocks(v_cmp, cmp_pos_v, cmp_w1_v, cmp_w2_v)
    nc = kc.shape[1]
    cmp_valid = (np.arange(nc) * CMP_STRIDE + CMP_LEN - 1)[None, :] <= t[:, None]
    s = jnp.einsum('bsgrd,bcgd->bgrsc', q, kc).astype(jnp.float32) * scale
    s = jnp.where(cmp_valid, s, NEG_INF)
    p_cmp = jnp.where(cmp_valid.any(axis=-1)[:, None], jax.nn.softmax(s, axis=-1), 0.0)
    o_cmp = jnp.einsum('bgrsc,bcgd->bsgrd', p_cmp.astype(vc.dtype), vc)
    nslc = seq // SLC_LEN
    imp = jnp.einsum('bgrsc,cj->bsgj', p_cmp, cmp_to_slc_matrix(nc, nslc))
    blk = np.arange(nslc)[None, :]
    cur = (t // SLC_LEN)[:, None]
    forced = (blk == 0) | ((blk <= cur) & (blk > cur - SLC_LOCAL))
    future = blk > cur
    imp = jnp.where(forced[:, None, :], FORCED_SCORE, imp)
    imp = jnp.where(future[:, None, :], NEG_INF, imp)
    _, sel = lax.top_k(imp, min(SLC_TOP, nslc))
    o_slc = selected_attention(q, k_slc, v_slc, sel, bias_table)
    o_win = banded_attention(q, k_win, v_win, bias_table, B_WINDOW)
    gts = jax.nn.sigmoid(gate_logits)
    return gts[..., 0:1] * o_cmp + gts[..., 1:2] * o_slc + gts[..., 2:3] * o_win


def hybrid_mixer(x, w_in, b_in, rel_bias, attn_sinks, cmp_pos_k, cmp_w1_k, cmp_w2_k,
                 cmp_pos_v, cmp_w1_v, cmp_w2_v, w_branch_a, w_branch_b, w_out):
    bsz, seq, _ = x.shape
    proj = x @ w_in + b_in
    splits = []
    off = 0
    for w in IN_WIDTHS[:-1]:
        off += w
        splits.append(off)
    (qa, ka, va, qb, kbc, vbc, kbs, vbs, kbw, vbw, nsa_g, merge_g) = jnp.split(proj, splits, axis=-1)

    def heads(z, g, r):
        return z.reshape(bsz, seq, g, r, HEAD_DIM)

    def kvh(z, g):
        return z.reshape(bsz, seq, g, HEAD_DIM)

    bias_a = rel_bias[:, :A_HEADS].reshape(N_BUCKETS, A_KV, A_REP)
    bias_b = rel_bias[:, A_HEADS:].reshape(N_BUCKETS, B_KV, B_REP)
    y_a = banded_attention(heads(qa, A_KV, A_REP), kvh(ka, A_KV), kvh(va, A_KV), bias_a,
                           A_WINDOW, attn_sinks.reshape(A_KV, A_REP))
    y_b = nsa_attention(heads(qb, B_KV, B_REP), kvh(kbc, B_KV), kvh(vbc, B_KV), kvh(kbs, B_KV),
                        kvh(vbs, B_KV), kvh(kbw, B_KV), kvh(vbw, B_KV),
                        nsa_g.reshape(bsz, seq, B_KV, B_REP, 3), bias_b,
                        cmp_pos_k, cmp_w1_k, cmp_w2_k, cmp_pos_v, cmp_w1_v, cmp_w2_v)
    gate_a, gate_b = jnp.split(jax.nn.sigmoid(merge_g), 2, axis=-1)
    merged = (gate_a * (y_a.reshape(bsz, seq, A_WIDTH) @ w_branch_a)
              + gate_b * (y_b.reshape(bsz, seq, B_WIDTH) @ w_branch_b))
    return merged @ w_out


def moe_ffn(x2d, w_router, b_router, w_gate_up, b_gate_up, w_down, b_down):
    n_tok, d = x2d.shape
    logits = (x2d @ w_router + b_router).astype(jnp.float32)
    top_vals, top_idx = lax.top_k(logits, TOP_K)
    gates = jax.nn.softmax(top_vals, axis=-1)
    flat_e = top_idx.reshape(-1)
    n_assign = flat_e.shape[0]
    order = jnp.argsort(flat_e)
    sorted_e = flat_e[order]
    tok = order // TOP_K
    counts = jnp.bincount(flat_e, length=N_EXPERTS)
    padded = (counts + MOE_BLOCK - 1) // MOE_BLOCK * MOE_BLOCK
    start = jnp.cumsum(counts) - counts
    padded_end = jnp.cumsum(padded)
    padded_start = padded_end - padded
    dest = padded_start[sorted_e] + jnp.arange(n_assign) - start[sorted_e]
    n_blocks = -(-n_assign // MOE_BLOCK) + N_EXPERTS
    rows = jnp.zeros((n_blocks * MOE_BLOCK, d), x2d.dtype).at[dest].set(x2d[tok])
    block_expert = jnp.minimum(jnp.searchsorted(padded_end, jnp.arange(n_blocks) * MOE_BLOCK, side='right'), N_EXPERTS - 1)

    def expert_block(args):
        xb, e = args
        h = xb @ w_gate_up[e] + b_gate_up[e]
        glu = jnp.minimum(h[:, :D_FF], SWIGLU_LIMIT)
        lin = jnp.clip(h[:, D_FF:], -SWIGLU_LIMIT, SWIGLU_LIMIT)
        act = glu * jax.nn.sigmoid(SWIGLU_ALPHA * glu) * (lin + 1.0)
        return act @ w_down[e] + b_down[e]

    out_rows = lax.map(expert_block, (rows.reshape(n_blocks, MOE_BLOCK, d), block_expert))
    out_rows = out_rows.reshape(n_blocks * MOE_BLOCK, d)
    weights = gates.reshape(-1)[order].astype(x2d.dtype)
    return jnp.zeros_like(x2d).at[tok].add(out_rows[dest] * weights[:, None])


def setup_inputs(seed: int = 0) -> dict:
    key = jax.random.key(seed)
    ks = jax.random.split(key, 24)

    def nrm(k, shape, scale):
        return jax.random.normal(k, shape, jnp.float32) * scale

    L = DEPTH
    fan_cmp = CMP_LEN * HEAD_DIM
    return {
        'x': nrm(ks[0], (BATCH, SEQ, D_MODEL), 1.0),
        'w_in': nrm(ks[1], (L, D_MODEL, IN_WIDTH), D_MODEL ** -0.5),
        'b_in': nrm(ks[2], (L, IN_WIDTH), 0.02),
        'rel_bias': nrm(ks[3], (N_BUCKETS, A_HEADS + B_HEADS), 0.3),
        'attn_sinks': nrm(ks[4], (L, A_HEADS), 0.5),
        'cmp_pos_k': nrm(ks[5], (L, CMP_LEN, HEAD_DIM), 0.1),
        'cmp_w1_k': nrm(ks[6], (L, fan_cmp, CMP_HIDDEN), fan_cmp ** -0.5),
        'cmp_w2_k': nrm(ks[7], (L, CMP_HIDDEN, HEAD_DIM), CMP_HIDDEN ** -0.5),
        'cmp_pos_v': nrm(ks[8], (L, CMP_LEN, HEAD_DIM), 0.1),
        'cmp_w1_v': nrm(ks[9], (L, fan_cmp, CMP_HIDDEN), fan_cmp ** -0.5),
        'cmp_w2_v': nrm(ks[10], (L, CMP_HIDDEN, HEAD_DIM), CMP_HIDDEN ** -0.5),
        'w_branch_a': nrm(ks[11], (L, A_WIDTH, D_MODEL), A_WIDTH ** -0.5),
        'w_branch_b': nrm(ks[12], (L, B_WIDTH, D_MODEL), B_WIDTH ** -0.5),
        'w_out': nrm(ks[13], (L, D_MODEL, D_MODEL), D_MODEL ** -0.5 * DEEPNORM_BETA),
        'ln1_g': 1.0 + nrm(ks[14], (L, D_MODEL), 0.05),
        'ln1_b': nrm(ks[15], (L, D_MODEL), 0.02),
        'w_router': nrm(ks[16], (L, D_MODEL, N_EXPERTS), D_MODEL ** -0.5),
        'b_router': nrm(ks[17], (L, N_EXPERTS), 0.01),
        'w_gate_up': nrm(ks[18], (L, N_EXPERTS, D_MODEL, 2 * D_FF), D_MODEL ** -0.5),
        'b_gate_up': nrm(ks[19], (L, N_EXPERTS, 2 * D_FF), 0.02),
        'w_down': nrm(ks[20], (L, N_EXPERTS, D_FF, D_MODEL), D_FF ** -0.5 * DEEPNORM_BETA),
        'b_down': nrm(ks[21], (L, N_EXPERTS, D_MODEL), 0.02),
        'ln2_g': 1.0 + nrm(ks[22], (L, D_MODEL), 0.05),
        'ln2_b': nrm(ks[23], (L, D_MODEL), 0.02),
    }


def reference(x, w_in, b_in, rel_bias, attn_sinks, cmp_pos_k, cmp_w1_k, cmp_w2_k,
              cmp_pos_v, cmp_w1_v, cmp_w2_v, w_branch_a, w_branch_b, w_out, ln1_g, ln1_b,
              w_router, b_router, w_gate_up, b_gate_up, w_down, b_down, ln2_g, ln2_b):
    bsz, seq, d = x.shape
    h = x
    for l in range(DEPTH):
        mix = hybrid_mixer(h, w_in[l], b_in[l], rel_bias, attn_sinks[l], cmp_pos_k[l], cmp_w1_k[l],
                           cmp_w2_k[l], cmp_pos_v[l], cmp_w1_v[l], cmp_w2_v[l], w_branch_a[l],
                           w_branch_b[l], w_out[l])
        h = layer_norm(DEEPNORM_ALPHA * h + mix, ln1_g[l], ln1_b[l])
        ffn = moe_ffn(h.reshape(bsz * seq, d), w_router[l], b_router[l], w_gate_up[l],
                      b_gate_up[l], w_down[l], b_down[l]).reshape(bsz, seq, d)
        h = layer_norm(DEEPNORM_ALPHA * h + ffn, ln2_g[l], ln2_b[l])
    return h
```

```python
import math
from collections import defaultdict
from contextlib import ExitStack
import numpy as np
import ml_dtypes
import concourse.bass as bass
import concourse.mybir as mybir
from concourse.bass_utils import run_bass_kernel_spmd

F32 = mybir.dt.float32
BF16 = mybir.dt.bfloat16
I32 = mybir.dt.int32
U32 = mybir.dt.uint32
AF = mybir.ActivationFunctionType
ALU = mybir.AluOpType

S = 2048
D = 1024
NT = 16
NEGM = -30000.0
ALPHA = 2.0 ** 0.25
LN_EPS = 1e-5
NE = 32


class _Op:
    __slots__ = ("eng", "fn", "waits", "signal", "track", "seq", "clock", "ninst", "isdma", "val")

    def __init__(self, eng, fn):
        self.eng = eng
        self.fn = fn
        self.waits = []
        self.signal = False
        self.ninst = 1
        self.isdma = False
        self.val = None


class Prog:
    ENGS = ("pe", "act", "dve", "pool", "sp")

    def __init__(self, nc):
        self.nc = nc
        self.ops = {e: [] for e in self.ENGS}
        self.known = {e: {} for e in self.ENGS}
        self.lastw = {}
        self.readers = defaultdict(list)
        self.track_ops = defaultdict(list)

    def _dep(self, op, d, kind):
        if d is None:
            return
        if d.track == op.eng and not op.isdma:
            if op.eng == "pe" or kind != "raw":
                return
        kn = self.known[op.eng]
        if kn.get(d.track, 0) >= d.seq:
            return
        op.waits.append(d)
        d.signal = True
        for t, s in d.clock.items():
            if kn.get(t, 0) < s:
                kn[t] = s

    def add(self, eng, fn, reads=(), writes=(), dma=None, ninst=1):
        op = _Op(eng, fn)
        op.ninst = ninst
        op.isdma = dma is not None
        op.track = ("dma:" + dma) if dma else eng
        for k in reads:
            self._dep(op, self.lastw.get(k), "raw")
        for k in writes:
            self._dep(op, self.lastw.get(k), "waw")
            for r in self.readers.get(k, ()):
                self._dep(op, r, "war")
        tl = self.track_ops[op.track]
        if op.isdma and tl:
            self._dep(op, tl[-1], "raw")
        op.seq = len(tl) + 1
        tl.append(op)
        ck = dict(self.known[op.eng])
        ck[op.track] = op.seq
        op.clock = ck
        for k in reads:
            self.readers[k].append(op)
        for k in writes:
            self.lastw[k] = op
            self.readers[k] = []
        self.ops[eng].append(op)
        return op

    def barrier(self):
        lasts = [tl[-1] for tl in self.track_ops.values() if tl]
        for e in self.ENGS:
            op = _Op(e, None)
            op.track = None
            kn = self.known[e]
            for d in lasts:
                if d.track == e and e == "pe":
                    continue
                if kn.get(d.track, 0) >= d.seq:
                    continue
                op.waits.append(d)
                d.signal = True
            self.ops[e].append(op)
        full = {d.track: d.seq for d in lasts}
        for e in self.ENGS:
            self.known[e] = dict(full)
        self.lastw = {}
        self.readers = defaultdict(list)

    def emit(self):
        nc = self.nc
        sems = {}
        for t, tl in self.track_ops.items():
            sems[t] = nc.alloc_semaphore("s_" + t.replace(":", "_"))
            c = 0
            for op in tl:
                if op.isdma:
                    c += 16 * op.ninst
                elif op.signal:
                    c += 1
                op.val = c
        engobj = {"pe": "tensor", "act": "scalar", "dve": "vector", "pool": "gpsimd", "sp": "sync"}
        lasts = [tl[-1] for tl in self.track_ops.values() if tl]
        with nc.Block() as block:
            for ename in self.ENGS:
                ops = self.ops[ename]

                def body(e, ops=ops, ename=ename):
                    for op in ops:
                        for d in op.waits:
                            e.wait_ge(sems[d.track], d.val)
                        if op.fn is None:
                            continue
                        r = op.fn(e)
                        if op.isdma:
                            rl = r if isinstance(r, (list, tuple)) else [r]
                            assert len(rl) == op.ninst
                            for ins in rl:
                                ins.then_inc(sems[op.track], 16)
                        elif op.signal:
                            ins = r[-1] if isinstance(r, (list, tuple)) else r
                            ins.then_inc(sems[op.track], 1)
                    if ename == "sp":
                        for d in lasts:
                            if d.val:
                                e.wait_ge(sems[d.track], d.val)

                getattr(block, engobj[ename])(body)


def _t5_bucket(rel):
    n = np.maximum(rel, 0)
    nf = np.maximum(n, 1).astype(np.float32)
    large = 16 + (np.log(nf / np.float32(16)) / np.float32(math.log(8.0)) * np.float32(16)).astype(np.int32)
    large = np.minimum(large, 31)
    return np.where(n < 16, n, large)


def _onehot(cls, J):
    oh = np.zeros((33, J), np.float32)
    oh[cls, np.arange(len(cls))] = 1.0
    return oh


def make_consts(CAP):
    bf = ml_dtypes.bfloat16
    c = {}
    c["c_identf"] = np.eye(128, dtype=np.float32)
    c["c_identb"] = np.eye(128, dtype=np.float32).astype(bf)
    c["c_antib"] = np.eye(128, dtype=np.float32)[::-1].copy().astype(bf)
    i = np.arange(384)
    rel = i - 127
    c["c_ohA"] = _onehot(np.where((rel < 0) | (rel >= 128), 32, _t5_bucket(rel)), 384)
    i = np.arange(768)
    rel = i - 127
    c["c_ohW"] = _onehot(np.where((rel < 0) | (rel >= 512), 32, _t5_bucket(rel)), 768)
    i = np.arange(640)
    rel = i + 1
    c["c_ohS"] = _onehot(_t5_bucket(rel), 640)
    cc = np.arange(127)[:, None]
    t = np.arange(S)[None, :]
    c["c_maskC"] = np.where(cc * 16 + 31 <= t, 0.0, NEGM).astype(bf)
    cs = np.arange(127)[:, None] * 16
    ss = np.arange(32)[None, :] * 64
    ov = np.clip(np.minimum(cs + 32, ss + 64) - np.maximum(cs, ss), 0, None)
    c["c_mcs"] = (ov / 32.0).astype(bf)
    tt = np.arange(S)
    cur = (tt // 64)[:, None]
    blk = np.arange(32)[None, :]
    forced = (blk == 0) | ((blk <= cur) & (blk > cur - 2))
    future = blk > cur
    nf = (~forced & ~future).astype(np.float32)
    addc = np.where(future, -100.0, np.where(forced, 100.0, 0.0)).astype(np.float32)

    def lay(a):
        a = a.reshape(16, 128, 32).transpose(1, 0, 2)
        return np.ascontiguousarray(np.repeat(a[:, :, None, :], 2, axis=2)).astype(np.float32).astype(bf)

    c["c_nf"] = lay(nf)
    c["c_addc"] = lay(addc)
    ex = np.zeros((32, 16, 128), np.float32)
    for kb in range(16):
        for m in range(128):
            ex[2 * kb + m // 64, kb, m] = 30000.0
    c["c_ex"] = ex.astype(bf)
    c["c_uut"] = np.triu(np.ones((128, 128), np.float32), 1).astype(bf)
    c["c_ones"] = np.ones((128, 128), np.float32).astype(bf)
    c["c_ecap"] = np.tile((np.arange(32, dtype=np.float32) * CAP)[None, :], (128, 1))
    return c


CONST_DT = {"c_identf": F32, "c_identb": BF16, "c_antib": BF16, "c_ohA": F32, "c_ohW": F32, "c_ohS": F32,
            "c_maskC": BF16, "c_mcs": BF16, "c_nf": BF16, "c_addc": BF16, "c_ex": BF16, "c_uut": BF16,
            "c_ones": BF16, "c_ecap": F32}


def build(NSEQ, CAP, dbg=False):
    nc = bass.Bass("TRN2", target_bir_lowering=False)
    P = Prog(nc)
    NTOK = NSEQ * S
    NTT = NTOK // 128
    consts = make_consts(CAP)

    def din(name, shape, dt=F32):
        return nc.dram_tensor(name, list(shape), dt, kind="ExternalInput")

    x_d = din("x", [NTOK, D])
    w_in = din("w_in", [D, 4120])
    b_in = din("b_in", [4120])
    rel_bias = din("rel_bias", [32, 16])
    sinks_d = din("attn_sinks", [8])
    pos_k = din("cmp_pos_k", [32, 64])
    w1_k = din("cmp_w1_k", [2048, 128])
    w2_k = din("cmp_w2_k", [128, 64])
    pos_v = din("cmp_pos_v", [32, 64])
    w1_v = din("cmp_w1_v", [2048, 128])
    w2_v = din("cmp_w2_v", [128, 64])
    wba_d = din("w_branch_a", [512, D])
    wbb_d = din("w_branch_b", [512, D])
    wout_d = din("w_out", [D, D])
    ln1g_d = din("ln1_g", [D])
    ln1b_d = din("ln1_b", [D])
    wr_d = din("w_router", [D, NE])
    br_d = din("b_router", [NE])
    wgu_d = din("w_gate_up", [NE, D, 2 * D])
    bgu_d = din("b_gate_up", [NE, 2 * D])
    wd_d = din("w_down", [NE, D, D])
    bd_d = din("b_down", [NE, D])
    ln2g_d = din("ln2_g", [D])
    ln2b_d = din("ln2_b", [D])
    cd = {k: din(k, v.shape, CONST_DT[k]) for k, v in consts.items()}
    out_d = nc.dram_tensor("out", [NTOK, D], F32, kind="ExternalOutput")
    if dbg:
        dbg_h = nc.dram_tensor("dbg_h", [NTOK, D], F32, kind="ExternalOutput")

    winc = nc.dram_tensor("winc", [128, 8, 4120], BF16, kind="Internal")
    w1c = nc.dram_tensor("w1c", [2, 128, 32, 128], BF16, kind="Internal")
    wbac = nc.dram_tensor("wbac", [128, 4, D], BF16, kind="Internal")
    wbbc = nc.dram_tensor("wbbc", [128, 4, D], BF16, kind="Internal")
    woutc = nc.dram_tensor("woutc", [128, 8, D], BF16, kind="Internal")
    gdA = nc.dram_tensor("gdA", [8, 384], F32, kind="Internal")
    gdW = nc.dram_tensor("gdW", [8, 768], F32, kind="Internal")
    gdS = nc.dram_tensor("gdS", [8, 640], F32, kind="Internal")
    hres = nc.dram_tensor("hres", [NTOK, D], F32, kind="Internal")
    xg = nc.dram_tensor("xg", [NE * CAP, D], BF16, kind="Internal")
    yg = nc.dram_tensor("yg", [NE * CAP, D], BF16, kind="Internal")

    uid = [0]

    def sbp(shape, dt, name=None):
        uid[0] += 1
        return nc.alloc_sbuf_tensor("%s_%d" % (name or "t", uid[0]), list(shape), dt)

    psS = [nc.alloc_psum_tensor("psS%d" % i, [128, 512], F32) for i in range(2)]
    psO = [nc.alloc_psum_tensor("psO%d" % i, [128, 4, 128], F32) for i in range(2)]
    psG = [nc.alloc_psum_tensor("psG%d" % i, [128, 512], F32) for i in range(2)]
    psT = [nc.alloc_psum_tensor("psT%d" % i, [128, 8, 128], BF16) for i in range(2)]
    rot = defaultdict(int)

    def nxt(lst, name):
        i = rot[name] % len(lst)
        rot[name] += 1
        return lst[i], (name, i)

    def mm(out, lhsT, rhs, start, stop, reads, writes):
        P.add("pe", lambda e: e.matmul(out, lhsT=lhsT, rhs=rhs, start=start, stop=stop, skip_group_check=True),
              reads=reads, writes=writes)

    def tr(out, in_, ident, reads, writes):
        P.add("pe", lambda e: e.transpose(out, in_, ident), reads=reads, writes=writes)

    def dma(eng, out, in_, reads, writes, sem):
        P.add(eng, lambda e: e.dma_start(out=out, in_=in_), reads=reads, writes=writes, dma=sem)

    def dve(fn, reads, writes):
        P.add("dve", fn, reads=reads, writes=writes)

    def act(fn, reads, writes):
        P.add("act", fn, reads=reads, writes=writes)

    def pool(fn, reads, writes):
        P.add("pool", fn, reads=reads, writes=writes)

    cst_i = [0]

    def ldc(out, in_, key, eng="sp"):
        cst_i[0] += 1
        dma(eng, out, in_, [], [key], "c%d" % (cst_i[0] % 4) if eng == "sp" else "cp%d" % (cst_i[0] % 2))

    STGN = 1024
    stg = [sbp([128, STGN], F32, "stg%d" % i) for i in range(2)]
    stgi = [0]

    cache_ready = [False]
    cli = [0]

    def ldcast(dst, src, wkeys, p0=0, ceng="pool", deng="sp", cache=None):
        if cache is not None and cache_ready[0]:
            cli[0] += 1
            dma(deng, dst, cache, ["wcache"], wkeys, "cl%d" % (cli[0] % 4))
            return
        shp = list(dst.shape)
        n = 1
        for d_ in shp[1:]:
            n *= d_
        if n > STGN:
            hsz = shp[1] // 2
            assert hsz * 2 == shp[1]
            ix = (slice(None), slice(0, hsz)) + (slice(None),) * (len(shp) - 2)
            iy = (slice(None), slice(hsz, shp[1])) + (slice(None),) * (len(shp) - 2)
            ldcast(dst[ix], src[ix], wkeys, p0, ceng, deng, cache[ix] if cache is not None else None)
            ldcast(dst[iy], src[iy], wkeys, p0, ceng, deng, cache[iy] if cache is not None else None)
            return
        i = stgi[0] % len(stg)
        stgi[0] += 1
        sv = stg[i][p0:p0 + shp[0], 0:n]
        if len(shp) == 3:
            sv = sv.rearrange("p (a b) -> p a b", b=shp[2])
        sk = ("stg", i)
        dma(deng, sv, src, [], [sk], "sg%d" % i)
        if ceng == "act":
            P.add("act", lambda e: e.copy(dst, sv), reads=[sk], writes=wkeys)
        else:
            P.add(ceng, lambda e: e.tensor_copy(dst, sv), reads=[sk], writes=wkeys)
        if cache is not None:
            cli[0] += 1
            dma("sp", cache, dst, wkeys, ["wcache"], "cs%d" % (cli[0] % 2))

    identf = sbp([128, 128], F32, "identf"); ldc(identf[:], cd["c_identf"].ap(), "identf")
    identb = sbp([128, 128], BF16, "identb"); ldc(identb[:], cd["c_identb"].ap(), "identb")
    antib = sbp([128, 128], BF16, "antib"); ldc(antib[:], cd["c_antib"].ap(), "antib")
    uut = sbp([128, 128], BF16, "uut"); ldc(uut[:], cd["c_uut"].ap(), "uut")
    onesb = sbp([128, 128], BF16, "onesb"); ldc(onesb[:], cd["c_ones"].ap(), "onesb")
    ecap = sbp([128, 32], F32, "ecap"); ldc(ecap[:], cd["c_ecap"].ap(), "ecap")
    c31bc = sbp([128, 16], F32, "c31bc"); ldc(c31bc[:], rel_bias[31:32, :].partition_broadcast(128), "c31bc")
    esink = sbp([128, 8], F32, "esink"); ldc(esink[:], sinks_d.ap().partition_broadcast(128), "esink")
    act(lambda e: e.activation(out=esink[:], in_=esink[:], func=AF.Exp), ["esink"], ["esink"])
    btok = sbp([128, 408], F32, "btok")
    for j, c0 in enumerate((640, 1664, 1920)):
        ldc(btok[:, j * 128:(j + 1) * 128], b_in[c0:c0 + 128].partition_broadcast(128), "btok")
    ldc(btok[:, 384:408], b_in[2048:2072].partition_broadcast(128), "btok")
    brt = sbp([128, 32], F32, "brt"); ldc(brt[:], br_d.ap().partition_broadcast(128), "brt")
    wr = sbp([128, 8, 32], BF16, "wr")
    ldcast(wr[:], wr_d.ap().rearrange("(kc p) n -> p kc n", p=128), ["wr"])
    w2kd = sbp([128, 128], BF16, "w2kd")
    ldcast(w2kd[:, 0:64], w2_k.ap(), ["w2kd"])
    ldcast(w2kd[:, 64:128], w2_k.ap(), ["w2kd"])
    w2vb = sbp([128, 64], BF16, "w2vb"); ldcast(w2vb[:], w2_v.ap(), ["w2vb"])
    gates_all = sbp([128, NTT, 4], F32, "gates_all")
    dest_all = sbp([128, NTT, 4], I32, "dest_all")
    tot = sbp([128, 32], F32, "tot")
    dve(lambda e: e.memset(tot[:], 0.0), [], ["tot"])
    bcol = sbp([128, 32], F32, "bcol")
    posT = sbp([64, 2, 32], BF16, "posT")
    bguT = sbp([128, NE * 16], F32, "bguT")
    mhalf = sbp([128, 1], F32, "mhalf")
    dve(lambda e: e.memset(mhalf[:], -0.5), [], ["mhalf"])
    epsb = sbp([128, 1], F32, "epsb")
    dve(lambda e: e.memset(epsb[:], LN_EPS), [], ["epsb"])
    btd = {"A": nc.dram_tensor("btdA", [128, 8, 256], BF16, kind="Internal"),
           "W": nc.dram_tensor("btdW", [128, 8, 640], BF16, kind="Internal"),
           "S": nc.dram_tensor("btdS", [128, 8, 512], BF16, kind="Internal")}
    with ExitStack() as es:
        def sbt(shape, dt, name):
            uid[0] += 1
            return es.enter_context(nc.sbuf_tensor("%s_%d" % (name, uid[0]), list(shape), dt))

        zrow = sbt([128, D], BF16, "zrow")
        dve(lambda e: e.memset(zrow[:], 0.0), [], ["zrow"])
        for e_ in range(NE):
            dma("sp", xg[e_ * CAP:(e_ + 1) * CAP, :].rearrange("(t p) d -> p t d", p=128),
                zrow[:, :].unsqueeze(1).to_broadcast([128, CAP // 128, D]), ["zrow"], ["xg"], "zx%d" % (e_ % 2))

        relext = sbt([33, 16], F32, "relext")
        BTA = sbt([128, 8, 256], BF16, "BTA")
        BTW = sbt([128, 8, 640], BF16, "BTW")
        BTS = sbt([128, 8, 512], BF16, "BTS")
        ldc(relext[0:32, :], rel_bias.ap(), "relext")
        dve(lambda e: e.memset(relext[32:33, :], NEGM), [], ["relext32"])
        for (vn, ohd, J, W, col0, gd, BT) in (("A", cd["c_ohA"], 384, 256, 0, gdA, BTA),
                                              ("W", cd["c_ohW"], 768, 640, 8, gdW, BTW),
                                              ("S", cd["c_ohS"], 640, 512, 8, gdS, BTS)):
            oh = sbt([33, J], F32, "oh" + vn)
            ldc(oh[:], ohd.ap(), "oh" + vn)
            Fv = sbt([8, J], F32, "Fv" + vn)
            for c0 in range(0, J, 512):
                n = min(512, J - c0)
                pb, pk = nxt(psG, "psG")
                mm(pb[0:8, 0:n], relext[0:33, col0:col0 + 8], oh[0:33, c0:c0 + n], True, True,
                   ["relext", "relext32", "oh" + vn], [pk])
                dve(lambda e, pb=pb, c0=c0, n=n, Fv=Fv: e.tensor_copy(Fv[:, c0:c0 + n], pb[0:8, 0:n]), [pk], ["Fv" + vn])
            dma("sp", gd.ap(), Fv[:], ["Fv" + vn], ["gd" + vn], "gd")
            for h in range(8):
                U = sbt([128, W], F32, "U%s%d" % (vn, h))
                Ub = sbt([128, W], BF16, "Ub%s%d" % (vn, h))
                uk = "U%s%d" % (vn, h)
                dma("sp", U[:], bass.AP(gd, h * J, [[1, 128], [1, W]]), ["gd" + vn], [uk], "u%d" % (h % 2))
                dve(lambda e, U=U, Ub=Ub: e.tensor_copy(Ub[:], U[:]), [uk], [uk + "b"])
                for c0 in range(0, W, 512):
                    n = min(512, W - c0)
                    pb, pk = nxt(psG, "psG")
                    mm(pb[:, 0:n], antib[:], Ub[:, c0:c0 + n], True, True, ["antib", uk + "b"], [pk])
                    dve(lambda e, pb=pb, c0=c0, n=n, BT=BT, h=h: e.tensor_copy(BT[:, h, c0:c0 + n], pb[:, 0:n]),
                        [pk], ["BT" + vn])
            dma("sp", btd[vn].ap(), BT[:], ["BT" + vn], ["btd" + vn], "gd")
        brow = sbt([32, 128], F32, "brow")
        ldc(brow[0:16, :], b_in[0:2048].rearrange("(c p) -> c p", p=128), "brow")
        ldc(brow[16:32, :], b_in[2072:4120].rearrange("(c p) -> c p", p=128), "brow")
        pb, pk = nxt(psG, "psG")
        tr(pb[:, 0:32], brow[0:32, :], identf[0:32, 0:32], ["brow", "identf"], [pk])
        dve(lambda e, pb=pb: e.tensor_copy(bcol[:], pb[:, 0:32]), [pk], ["bcol"])
        for kv, pd in enumerate((pos_k, pos_v)):
            pr = sbt([32, 64], F32, "posr%d" % kv)
            ldc(pr[:], pd.ap(), "posr%d" % kv)
            pb, pk = nxt(psG, "psG")
            tr(pb[0:64, 0:32], pr[0:32, :], identf[0:32, 0:32], ["posr%d" % kv, "identf"], [pk])
            dve(lambda e, pb=pb, kv=kv: e.tensor_copy(posT[:, kv, :], pb[0:64, 0:32]), [pk], ["posT"])
        bgv = bgu_d.ap().rearrange("e (c p) -> (e c) p", p=128)
        for r in range(4):
            bt_ = sbt([128, 128], F32, "bgr%d" % r)
            ldc(bt_[:], bgv[r * 128:(r + 1) * 128, :], "bgr%d" % r)
            pb, pk = nxt(psG, "psG")
            tr(pb[:, 0:128], bt_[:], identf[:], ["bgr%d" % r, "identf"], [pk])
            dve(lambda e, pb=pb, r=r: e.tensor_copy(bguT[:, r * 128:(r + 1) * 128], pb[:, 0:128]), [pk], ["bguT"])
        P.barrier()

    es_seq = ExitStack()

    def sbq(shape, dt, name):
        uid[0] += 1
        return es_seq.enter_context(nc.sbuf_tensor("%s_%d" % (name, uid[0]), list(shape), dt))

    xT = sbq([128, 8, S], BF16, "xT")
    ya = sbq([128, NT, 512], BF16, "ya")
    yb = sbq([128, NT, 512], BF16, "yb")
    wcols = {"qA": 0, "kA": 512, "vA": 640, "qB": 768, "kBc": 1280, "vBc": 1408, "kBs": 1536, "vBs": 1664,
             "kBw": 1792, "vBw": 1920, "ng": 2048, "mg": 2072}

    def w_in_view(c0, n):
        return w_in[:, c0:c0 + n].rearrange("(kc p) n -> p kc n", p=128)

    for sq in range(NSEQ):
        tok0 = sq * S
        with ExitStack() as es:
            def sbt(shape, dt, name):
                uid[0] += 1
                return es.enter_context(nc.sbuf_tensor("%s_%d" % (name, uid[0]), list(shape), dt))

            xld = [sbt([128, D], BF16, "xld%d" % i) for i in range(2)]
            maskC = sbt([128, S], BF16, "maskC"); ldc(maskC[0:127, :], cd["c_maskC"].ap(), "maskC")
            nfm = sbt([128, 16, 2, 32], BF16, "nfm"); ldc(nfm[:], cd["c_nf"].ap(), "nfm")
            addc = sbt([128, 16, 2, 32], BF16, "addc"); ldc(addc[:], cd["c_addc"].ap(), "addc")
            exm = sbt([128, 16, 128], BF16, "exm")
            pool(lambda e: e.memset(exm[:], 0.0), [], ["exm"])
            ldc(exm[0:32, :, :], cd["c_ex"].ap(), "exm")
            BTA = sbt([128, 8, 256], BF16, "BTA"); ldc(BTA[:], btd["A"].ap(), "BTA")
            BTW = sbt([128, 8, 640], BF16, "BTW"); ldc(BTW[:], btd["W"].ap(), "BTW")
            wst = [sbt([128, 8, 256], BF16, "wst%d" % i) for i in range(2)]
            QT = sbt([128, 4, S], BF16, "QT")
            K1 = sbt([128, 2, 2, S], BF16, "K1")
            K2 = sbt([128, 2, 2, S], BF16, "K2")
            dve(lambda e: e.memset(K1[:], 0.0), [], ["K1"])
            dve(lambda e: e.memset(K2[:], 0.0), [], ["K2"])
            V1 = sbt([128, NT, 2, 65], BF16, "V1")
            V2 = sbt([128, NT, 2, 65], BF16, "V2")
            Gt = sbt([128, NT, 24], F32, "Gt")
            KcT = sbt([128, 2, 2, 128], BF16, "KcT")
            dve(lambda e: e.memset(KcT[:], 0.0), [], ["KcT"])
            VcE = sbt([128, 2, 97], BF16, "VcE")
            acc = sbt([128, 4, 512], F32, "acc")
            KC = acc[:, :, :].rearrange("p a b -> p (a b)").bitcast(BF16).rearrange("p (j s) -> p j s", j=2)
            pTs = [sbt([128, 512], BF16, "pT%d" % i) for i in range(5)]
            imp = sbt([128, 4, 2, 32], F32, "imp")
            impm = sbt([128, 4, 2, 32], F32, "impm")
            selb = sbt([128, 4, 2, 32], BF16, "selb")
            selT = sbt([128, 2, 512], BF16, "selT")
            pool(lambda e: e.memset(selT[:], 0.0), [], ["selT"])
            m8 = sbt([128, 8, 8], F32, "m8")
            den = sbt([128, 4], F32, "den")
            rec = sbt([128, 4], F32, "rec")
            recg = sbt([128, 4], F32, "recg")
            gx = sbt([128, 128], F32, "gx")
            gt_ = sbt([128, 128], F32, "gt")
            gs = sbt([128, 128], F32, "gs")
            hact = sbt([128, 128], BF16, "hact")
            posb = sbt([128, 2], F32, "posb")

            for t in range(NT):
                xl = xld[t % 2]
                xk = ("xld", t % 2)
                ldcast(xl[:], x_d[tok0 + t * 128: tok0 + (t + 1) * 128, :], [xk], ceng="act")
                pb, pk = nxt(psT, "psT")
                for kc in range(8):
                    tr(pb[:, kc, :], xl[:, kc * 128:(kc + 1) * 128], identb[:], [xk, "identb"], [pk])
                dve(lambda e, pb=pb, t=t: e.tensor_copy(xT[:, :, t * 128:(t + 1) * 128], pb[:]), [pk], [("xT", t)])
            xTkeys = [("xT", t) for t in range(NT)]

            wsti = [0]

            def load_w(c0, n, dup64=False):
                i = wsti[0] % 2
                wsti[0] += 1
                wt = wst[i]
                wk = ("wst", i)
                if dup64:
                    for j in range(4):
                        src = c0 + (j // 2) * 64
                        ldcast(wt[:, :, j * 64:(j + 1) * 64], w_in_view(src, 64), [wk], cache=winc[:, :, src:src + 64])
                else:
                    ldcast(wt[:, :, 0:n], w_in_view(c0, n), [wk], cache=winc[:, :, c0:c0 + n])
                return wt, wk

            def proj_fm(dst, dkey, wt, wk, ncols, bcs, scale):
                for j in range(ncols // 128):
                    for n4 in range(4):
                        pb, pk = nxt(psG, "psG")
                        for kc in range(8):
                            mm(pb[:, :], wt[:, kc, j * 128:(j + 1) * 128], xT[:, kc, n4 * 512:(n4 + 1) * 512],
                               kc == 0, kc == 7, [wk] + xTkeys[n4 * 4:(n4 + 1) * 4], [pk])
                        bc = bcs[j]
                        dve(lambda e, pb=pb, j=j, n4=n4, bc=bc, dst=dst: e.tensor_scalar(
                            out=dst(j)[:, n4 * 512:(n4 + 1) * 512], in0=pb[:, :], scalar1=bcol[:, bc:bc + 1], scalar2=scale,
                            op0=ALU.add, op1=ALU.mult), [pk, "bcol"], [dkey])

            def proj_q(c0, bc0):
                for half in range(2):
                    wt, wk = load_w(c0 + half * 256, 256)
                    proj_fm(lambda j, half=half: QT[:, half * 2 + j, :], "QT", wt, wk, 256,
                            [bc0 + half * 2, bc0 + half * 2 + 1], 0.125)

            def proj_kdup(Kt, kkey, c0, bc):
                wt, wk = load_w(c0, 256, dup64=True)
                for g in range(2):
                    for n4 in range(4):
                        pb, pk = nxt(psG, "psG")
                        for kc in range(8):
                            mm(pb[:, :], wt[:, kc, g * 128:(g + 1) * 128], xT[:, kc, n4 * 512:(n4 + 1) * 512],
                               kc == 0, kc == 7, [wk] + xTkeys[n4 * 4:(n4 + 1) * 4], [pk])
                        for hh in range(2):
                            dve(lambda e, pb=pb, g=g, n4=n4, hh=hh, Kt=Kt: e.tensor_scalar(
                                out=Kt[hh * 64:(hh + 1) * 64, g, hh, n4 * 512:(n4 + 1) * 512], in0=pb[hh * 64:(hh + 1) * 64, :],
                                scalar1=bdup[hh * 64:(hh + 1) * 64, g:g + 1], scalar2=None, op0=ALU.add), [pk, "bdup"], [kkey])

            bdup = sbt([128, 2], F32, "bdup")

            def make_bdup(bc):
                for g in range(2):
                    for hh in range(2):
                        dma("sp", bdup[hh * 64:(hh + 1) * 64, g:g + 1],
                            b_in[bc * 128 + g * 64:bc * 128 + g * 64 + 64].rearrange("(p o) -> p o", o=1),
                            [], ["bdup"], "bd")

            def proj_tok(c0, Vt, vkey, boff, withg):
                n = 152 if withg else 128
                i = wsti[0] % 2
                wsti[0] += 1
                wt = wst[i]
                wk = ("wst", i)
                ldcast(wt[:, :, 0:128], w_in_view(c0, 128), [wk], cache=winc[:, :, c0:c0 + 128])
                if withg:
                    ldcast(wt[:, :, 128:152], w_in_view(2048, 24), [wk], cache=winc[:, :, 2048:2072])
                pool(lambda e: e.memset(Vt[:, :, :, 64:65], 1.0), [], [vkey])
                for t in range(NT):
                    pb, pk = nxt(psG, "psG")
                    for kc in range(8):
                        mm(pb[:, 0:n], xT[:, kc, t * 128:(t + 1) * 128], wt[:, kc, 0:n], kc == 0, kc == 7,
                           [wk, ("xT", t)], [pk])
                    dve(lambda e, pb=pb, t=t: e.tensor_tensor(
                        out=Vt[:, t, :, 0:64], in0=pb[:, 0:128].rearrange("p (g d) -> p g d", g=2),
                        in1=btok[:, boff:boff + 128].rearrange("p (g d) -> p g d", g=2), op=ALU.add), [pk, "btok"], [vkey])
                    if withg:
                        dve(lambda e, pb=pb, t=t: e.tensor_tensor(out=Gt[:, t, :], in0=pb[:, 128:152], in1=btok[:, 384:408],
                                                                   op=ALU.add), [pk, "btok"], ["Gt"])
                if withg:
                    act(lambda e: e.activation(out=Gt[:], in_=Gt[:], func=AF.Sigmoid), ["Gt"], ["Gt"])

            def attn_pv(po, pok, pT, pTk, subs, Vt, vkey, kb, g, ncol, first):
                for s_ in subs:
                    mm(po[:, s_, 0:ncol], pT[:, s_ * 128:(s_ + 1) * 128], Vt[:, kb, g, 0:ncol] if Vt is not None else None,
                       first[0], True, [pTk, vkey], [pok])
                    first[0] = False

            SB = [(psS[0], ("psS", 0)), (psS[1], ("psS", 1)), (psG[0], ("psG", 0)), (psG[1], ("psG", 1))]

            def nxtS():
                i = rot["SB"] % 4
                rot["SB"] += 1
                return SB[i]

            def run_tiles(tiles, lag=3):
                n = len(tiles)
                for i in range(n + lag):
                    if i < n:
                        tiles[i][0]()
                    j = i - lag
                    if j >= 0:
                        tiles[j][1]()

            def mk_group():
                return {"po": None, "pok": None, "first": [True]}

            def grp_po(grp):
                if grp["po"] is None:
                    grp["po"], grp["pok"] = nxt(psO, "psO")
                return grp["po"], grp["pok"]

            proj_q(wcols["qA"], 0)
            make_bdup(4)
            proj_kdup(K1, "K1", wcols["kA"], 4)
            proj_tok(wcols["vA"], V1, "V1", 0, False)
            tilesA = []
            for Q in range(4):
                for h in range(8):
                    grp = mk_group()
                    dls = [dl for dl in range(-1, 4) if 4 * Q + dl >= 0]
                    for dl in dls:
                        st = {}

                        def fs(Q=Q, h=h, dl=dl, st=st):
                            g = h // 4
                            hb = (h % 2) * 64
                            kb = 4 * Q + dl
                            subs = [s_ for s_ in (dl, dl + 1) if 0 <= s_ <= 3]
                            c0, c1 = subs[0] * 128, (subs[-1] + 1) * 128
                            ps, psk = nxtS()
                            mm(ps[:, c0:c1], K1[:, g, h % 2, kb * 128:(kb + 1) * 128],
                               QT[:, h // 2, Q * 512 + c0:Q * 512 + c1], True, False, ["K1", "QT"], [psk])
                            mm(ps[:, c0:c1], identb[:], BTA[:, h, c0 - 128 * dl:c1 - 128 * dl], False, True,
                               ["identb", "BTA"], [psk])
                            pT, pTk = nxt(pTs, "pT")
                            act(lambda e, ps=ps, pT=pT, c0=c0, c1=c1: e.activation(out=pT[:, c0:c1], in_=ps[:, c0:c1], func=AF.Exp),
                                [psk], [pTk])
                            st.update(pT=pT, pTk=pTk, subs=subs, kb=kb, g=g)

                        def fp(Q=Q, h=h, st=st, grp=grp, last=(dl == dls[-1])):
                            po, pok = grp_po(grp)
                            attn_pv(po, pok, st["pT"], st["pTk"], st["subs"], V1, "V1", st["kb"], st["g"], 65, grp["first"])
                            if last:
                                dve(lambda e, po=po, h=h: e.tensor_scalar(out=den[:], in0=po[:, :, 64], scalar1=esink[:, h:h + 1],
                                                                          scalar2=None, op0=ALU.add), [pok, "esink"], ["den"])
                                dve(lambda e: e.reciprocal(rec[:], den[:]), ["den"], ["rec"])
                                dve(lambda e, po=po, h=h, Q=Q: e.tensor_tensor(
                                    out=ya[:, 4 * Q:4 * Q + 4, h * 64:(h + 1) * 64], in0=po[:, :, 0:64],
                                    in1=rec[:, :].unsqueeze(2).to_broadcast([128, 4, 64]), op=ALU.mult), [pok, "rec"], ["ya"])

                        tilesA.append((fs, fp))
            run_tiles(tilesA)

            proj_q(wcols["qB"], 6)
            make_bdup(12)
            proj_kdup(K1, "K1", wcols["kBs"], 12)
            make_bdup(14)
            proj_kdup(K2, "K2", wcols["kBw"], 14)
            proj_tok(wcols["vBs"], V1, "V1", 128, True)
            proj_tok(wcols["vBw"], V2, "V2", 256, False)
            wt, wk = load_w(wcols["kBc"], 256)
            proj_fm(lambda j: KC[:, j, :], "acc", wt, wk, 256, [10, 11], 1.0)
            pool(lambda e: e.memset(VcE[:, :, 64:65], 1.0), [], ["VcE"])
            for g in range(2):
                dma("sp", VcE[0:127, g, 65:97], cd["c_mcs"].ap(), [], ["VcE"], "mcs")
            for kv, w1d in enumerate((w1_k, w1_v)):
                i = wsti[0] % 2
                wsti[0] += 1
                w1t = wst[i]
                wk = ("wst", i)
                w1v = w1t[:, :, :].rearrange("p a b -> p (a b)")
                for lh in range(2):
                    if lh == 1:
                        i = wsti[0] % 2
                        wsti[0] += 1
                        w1t = wst[i]
                        wk = ("wst", i)
                        w1v = w1t[:, :, :].rearrange("p a b -> p (a b)")
                    src = w1d[lh * 1024:(lh + 1) * 1024, :].rearrange("(l d) h -> d l h", d=64)
                    for hh in range(2):
                        ldcast(w1v[hh * 64:(hh + 1) * 64, :].rearrange("p (l h) -> p l h", h=128), src, [wk], p0=hh * 64,
                               cache=w1c[kv, hh * 64:(hh + 1) * 64, lh * 16:(lh + 1) * 16, :])
                    pb2 = psG[0]
                    for l in range(16):
                        mm(pb2[:, 500 + kv:501 + kv], w1v[0:64, l * 128:(l + 1) * 128], posT[0:64, kv, lh * 16 + l:lh * 16 + l + 1],
                           (lh == 0 and l == 0), (lh == 1 and l == 15), [wk, "posT"], [("psG", 0)])
                    for g in range(2):
                        pH, pHk = psO[g], ("psO", g)
                        for l in range(16):
                            la = lh * 16 + l
                            mm(pH[:, 0, 0:127], w1v[g * 64:(g + 1) * 64, l * 128:(l + 1) * 128],
                               KC[g * 64:(g + 1) * 64, kv, la:la + 16 * 126 + 1:16], (la == 0), (la == 31), [wk, "acc"], [pHk])
                dve(lambda e, kv=kv: e.tensor_copy(posb[:, kv:kv + 1], psG[0][:, 500 + kv:501 + kv]), [("psG", 0)], ["posb"])
                for g in range(2):
                    pH, pHk = psO[g], ("psO", g)
                    act(lambda e, pH=pH, kv=kv: e.activation(out=gx[:, 0:127], in_=pH[:, 0, 0:127], func=AF.Identity,
                                                             bias=posb[:, kv:kv + 1], scale=1.0), [pHk, "posb"], ["gx"])
                    dve(lambda e: e.tensor_tensor(out=gt_[:, 0:127], in0=gx[:, 0:127], in1=gx[:, 0:127], op=ALU.mult), ["gx"], ["gt"])
                    dve(lambda e: e.tensor_scalar(out=gt_[:, 0:127], in0=gt_[:, 0:127], scalar1=0.044715, scalar2=1.0,
                                                  op0=ALU.mult, op1=ALU.add), ["gt"], ["gt"])
                    dve(lambda e: e.tensor_tensor(out=gt_[:, 0:127], in0=gt_[:, 0:127], in1=gx[:, 0:127], op=ALU.mult), ["gt", "gx"], ["gt"])
                    act(lambda e: e.activation(out=gs[:, 0:127], in_=gt_[:, 0:127], func=AF.Sigmoid, scale=1.5957691216057308),
                        ["gt"], ["gs"])
                    dve(lambda e: e.tensor_tensor(out=hact[:, 0:127], in0=gx[:, 0:127], in1=gs[:, 0:127], op=ALU.mult),
                        ["gx", "gs"], ["hact"])
                    pb, pk = nxt(psG, "psG")
                    if kv == 0:
                        mm(pb[:, 0:127], w2kd[:], hact[:, 0:127], True, True, ["w2kd", "hact"], [pk])
                        for hh in range(2):
                            dve(lambda e, pb=pb, g=g, hh=hh: e.tensor_copy(KcT[hh * 64:(hh + 1) * 64, g, hh, 0:127], pb[hh * 64:(hh + 1) * 64, 0:127]),
                                [pk], ["KcT"])
                    else:
                        mm(pb[0:127, 0:64], hact[:, 0:127], w2vb[:], True, True, ["w2vb", "hact"], [pk])
                        dve(lambda e, pb=pb, g=g: e.tensor_copy(VcE[0:127, g, 0:64], pb[0:127, 0:64]), [pk], ["VcE"])

            for Q in range(4):
                tilesC = []
                for h in range(8):
                    st = {}

                    def fs(Q=Q, h=h, st=st):
                        g = h // 4
                        hb = (h % 2) * 64
                        ps, psk = nxtS()
                        mm(ps[0:127, :], KcT[:, g, h % 2, 0:127], QT[:, h // 2, Q * 512:(Q + 1) * 512], True, False,
                           ["KcT", "QT"], [psk])
                        mm(ps[0:127, :], identb[0:127, 0:127], maskC[0:127, Q * 512:(Q + 1) * 512], False, True,
                           ["identb", "maskC"], [psk])
                        pT, pTk = nxt(pTs, "pT")
                        act(lambda e, ps=ps, pT=pT: e.activation(out=pT[0:127, :], in_=ps[0:127, :], func=AF.Exp), [psk], [pTk])
                        st.update(pT=pT, pTk=pTk)

                    def fp(Q=Q, h=h, st=st):
                        g = h // 4
                        pT, pTk = st["pT"], st["pTk"]
                        po, pok = nxt(psO, "psO")
                        for s_ in range(4):
                            mm(po[:, s_, 0:97], pT[0:127, s_ * 128:(s_ + 1) * 128], VcE[0:127, g, 0:97], s_ == 0, True,
                               [pTk, "VcE"], [pok])
                        dve(lambda e, po=po: e.tensor_scalar(out=den[:], in0=po[:, :, 64], scalar1=1e-30, scalar2=None, op0=ALU.add),
                            [pok], ["den"])
                        dve(lambda e: e.reciprocal(rec[:], den[:]), ["den"], ["rec"])
                        dve(lambda e, h=h, Q=Q: e.tensor_tensor(out=recg[:], in0=rec[:], in1=Gt[:, 4 * Q:4 * Q + 4, h * 3 + 0], op=ALU.mult),
                            ["rec", "Gt"], ["recg"])
                        dve(lambda e, po=po, h=h: e.tensor_tensor(out=acc[:, :, h * 64:(h + 1) * 64], in0=po[:, :, 0:64],
                                                                  in1=recg[:, :].unsqueeze(2).to_broadcast([128, 4, 64]), op=ALU.mult),
                            [pok, "recg"], ["acc"])
                        if h % 4 == 0:
                            dve(lambda e, po=po, g=g: e.tensor_tensor(out=imp[:, :, g, :], in0=po[:, :, 65:97],
                                                                      in1=rec[:, :].unsqueeze(2).to_broadcast([128, 4, 32]), op=ALU.mult),
                                [pok, "rec"], ["imp"])
                        else:
                            dve(lambda e, po=po, g=g: e.tensor_tensor(out=impm[:, :, g, :], in0=po[:, :, 65:97],
                                                                      in1=rec[:, :].unsqueeze(2).to_broadcast([128, 4, 32]), op=ALU.mult),
                                [pok, "rec"], ["impm"])
                            dve(lambda e, g=g: e.tensor_tensor(out=imp[:, :, g, :], in0=imp[:, :, g, :], in1=impm[:, :, g, :], op=ALU.add),
                                ["imp", "impm"], ["imp"])

                    tilesC.append((fs, fp))
                run_tiles(tilesC)
                dve(lambda e, Q=Q: e.tensor_tensor(out=impm[:], in0=imp[:], in1=nfm[:, 4 * Q:4 * Q + 4, :, :], op=ALU.mult),
                    ["imp", "nfm"], ["impm"])
                dve(lambda e, Q=Q: e.tensor_tensor(out=impm[:], in0=impm[:], in1=addc[:, 4 * Q:4 * Q + 4, :, :], op=ALU.add),
                    ["impm", "addc"], ["impm"])
                for s_ in range(4):
                    for g in range(2):
                        dve(lambda e, s_=s_, g=g: e.max(m8[:, s_ * 2 + g, :], impm[:, s_, g, :]), ["impm"], ["m8"])
                for s_ in range(4):
                    for g in range(2):
                        dve(lambda e, s_=s_, g=g: e.tensor_scalar(out=selb[:, s_, g, :], in0=impm[:, s_, g, :],
                                                                  scalar1=m8[:, s_ * 2 + g, 7:8], scalar2=1.0,
                                                                  op0=ALU.is_ge, op1=ALU.subtract), ["impm", "m8"], ["selb"])
                pbt, pbtk = nxt(psT, "psT")
                for g in range(2):
                    for s_ in range(4):
                        tr(pbt[0:32, g * 4 + s_, :], selb[:, s_, g, :], identb[:], ["selb", "identb"], [pbtk])
                dve(lambda e, pbt=pbt: e.tensor_copy(selT[0:32, :, :], pbt[0:32, :, :].rearrange("p (g s) q -> p g (s q)", g=2)),
                    [pbtk], ["selT"])
                tilesB = []
                for h in range(8):
                    grp = mk_group()
                    dls = [dl for dl in range(-4, 4) if 4 * Q + dl >= 0]
                    for dl in dls:
                        st = {}

                        def fs(Q=Q, h=h, dl=dl, st=st):
                            g = h // 4
                            hb = (h % 2) * 64
                            kb = 4 * Q + dl
                            subs = [s_ for s_ in range(4) if dl <= s_ <= dl + 4]
                            c0, c1 = subs[0] * 128, (subs[-1] + 1) * 128
                            ps, psk = nxtS()
                            mm(ps[:, c0:c1], K2[:, g, h % 2, kb * 128:(kb + 1) * 128],
                               QT[:, h // 2, Q * 512 + c0:Q * 512 + c1], True, False, ["K2", "QT"], [psk])
                            mm(ps[:, c0:c1], identb[:], BTW[:, h, c0 - 128 * dl:c1 - 128 * dl], False, True, ["identb", "BTW"], [psk])
                            pT, pTk = nxt(pTs, "pT")
                            act(lambda e, ps=ps, pT=pT, c0=c0, c1=c1: e.activation(out=pT[:, c0:c1], in_=ps[:, c0:c1], func=AF.Exp),
                                [psk], [pTk])
                            st.update(pT=pT, pTk=pTk, subs=subs, kb=kb, g=g)

                        def fp(Q=Q, h=h, st=st, grp=grp, last=(dl == dls[-1])):
                            po, pok = grp_po(grp)
                            attn_pv(po, pok, st["pT"], st["pTk"], st["subs"], V2, "V2", st["kb"], st["g"], 65, grp["first"])
                            if last:
                                dve(lambda e, po=po: e.reciprocal(rec[:], po[:, :, 64]), [pok], ["rec"])
                                dve(lambda e, h=h, Q=Q: e.tensor_tensor(out=recg[:], in0=rec[:], in1=Gt[:, 4 * Q:4 * Q + 4, h * 3 + 2],
                                                                        op=ALU.mult), ["rec", "Gt"], ["recg"])
                                for s_ in range(4):
                                    dve(lambda e, po=po, h=h, s_=s_: e.scalar_tensor_tensor(
                                        out=acc[:, s_, h * 64:(h + 1) * 64], in0=po[:, s_, 0:64], scalar=recg[:, s_:s_ + 1],
                                        in1=acc[:, s_, h * 64:(h + 1) * 64], op0=ALU.mult, op1=ALU.add), [pok, "recg", "acc"], ["acc"])

                        tilesB.append((fs, fp))
                for h in range(8):
                    grp = mk_group()
                    kbs = list(range(0, 4 * Q + 4))
                    for kb in kbs:
                        st = {}

                        def fs(Q=Q, h=h, kb=kb, st=st):
                            g = h // 4
                            hb = (h % 2) * 64
                            dl = kb - 4 * Q
                            subs = [s_ for s_ in range(4) if s_ >= dl]
                            c0 = subs[0] * 128
                            ps, psk = nxtS()
                            mm(ps[:, c0:512], K1[:, g, h % 2, kb * 128:(kb + 1) * 128],
                               QT[:, h // 2, Q * 512 + c0:(Q + 1) * 512], True, False, ["K1", "QT"], [psk])
                            mm(ps[:, c0:512], exm[:, kb, :], selT[:, g, c0:512], False, dl < -1, ["exm", "selT"], [psk])
                            if dl == -1:
                                mm(ps[:, 0:384], identb[:], BTW[:, h, 128:512], False, False, ["identb", "BTW"], [psk])
                                mm(ps[:, 384:512], identb[:], BTW[:, h, 256:384], False, True, ["identb", "BTW"], [psk])
                            elif dl >= 0:
                                mm(ps[:, c0:512], identb[:], BTW[:, h, c0 - 128 * dl:512 - 128 * dl], False, True,
                                   ["identb", "BTW"], [psk])
                            pT, pTk = nxt(pTs, "pT")
                            if dl < -1:
                                act(lambda e, ps=ps, pT=pT, c0=c0, h=h: e.activation(out=pT[:, c0:512], in_=ps[:, c0:512], func=AF.Exp,
                                                                                     bias=c31bc[:, 8 + h:9 + h], scale=1.0),
                                    [psk, "c31bc"], [pTk])
                            else:
                                act(lambda e, ps=ps, pT=pT, c0=c0: e.activation(out=pT[:, c0:512], in_=ps[:, c0:512], func=AF.Exp),
                                    [psk], [pTk])
                            st.update(pT=pT, pTk=pTk, subs=subs, g=g)

                        def fp(Q=Q, h=h, kb=kb, st=st, grp=grp, last=(kb == kbs[-1])):
                            po, pok = grp_po(grp)
                            attn_pv(po, pok, st["pT"], st["pTk"], st["subs"], V1, "V1", kb, st["g"], 65, grp["first"])
                            if last:
                                dve(lambda e, po=po: e.reciprocal(rec[:], po[:, :, 64]), [pok], ["rec"])
                                dve(lambda e, h=h, Q=Q: e.tensor_tensor(out=recg[:], in0=rec[:], in1=Gt[:, 4 * Q:4 * Q + 4, h * 3 + 1],
                                                                        op=ALU.mult), ["rec", "Gt"], ["recg"])
                                for s_ in range(4):
                                    dve(lambda e, po=po, h=h, s_=s_: e.scalar_tensor_tensor(
                                        out=acc[:, s_, h * 64:(h + 1) * 64], in0=po[:, s_, 0:64], scalar=recg[:, s_:s_ + 1],
                                        in1=acc[:, s_, h * 64:(h + 1) * 64], op0=ALU.mult, op1=ALU.add), [pok, "recg", "acc"], ["acc"])

                        tilesB.append((fs, fp))
                run_tiles(tilesB)
                pool(lambda e, Q=Q: e.tensor_copy(yb[:, 4 * Q:4 * Q + 4, :], acc[:]), ["acc"], ["yb"])
            P.barrier()

        with ExitStack() as es:
            def sbt(shape, dt, name):
                uid[0] += 1
                return es.enter_context(nc.sbuf_tensor("%s_%d" % (name, uid[0]), list(shape), dt))

            ln1g = sbt([128, D], F32, "ln1g"); ldc(ln1g[:], ln1g_d.ap().partition_broadcast(128), "ln1g")
            ln1b = sbt([128, D], F32, "ln1b"); ldc(ln1b[:], ln1b_d.ap().partition_broadcast(128), "ln1b")
            wba = sbt([128, 4, D], BF16, "wba")
            wbb = sbt([128, 4, D], BF16, "wbb")
            wout = sbt([128, 8, D], BF16, "wout")
            wg = sbt([128, 8, 2048], BF16, "wg")
            import itertools
            cvt = itertools.cycle(("act", "dve"))
            for k2 in range(2):
                ldcast(wba[:, 2 * k2:2 * k2 + 2, :], wba_d.ap().rearrange("(kc p) n -> p kc n", p=128)[:, 2 * k2:2 * k2 + 2, :], ["wba"],
                       ceng=next(cvt), cache=wbac[:, 2 * k2:2 * k2 + 2, :])
                ldcast(wbb[:, 2 * k2:2 * k2 + 2, :], wbb_d.ap().rearrange("(kc p) n -> p kc n", p=128)[:, 2 * k2:2 * k2 + 2, :], ["wbb"],
                       ceng=next(cvt), cache=wbbc[:, 2 * k2:2 * k2 + 2, :])
            for k2 in range(4):
                ldcast(wout[:, 2 * k2:2 * k2 + 2, :], wout_d.ap().rearrange("(kc p) n -> p kc n", p=128)[:, 2 * k2:2 * k2 + 2, :], ["wout"],
                       ceng=next(cvt), cache=woutc[:, 2 * k2:2 * k2 + 2, :])
            for q4 in range(4):
                for h4 in range(2):
                    ldcast(wg[:, 4 * h4:4 * h4 + 4, q4 * 512:(q4 + 1) * 512], w_in_view(2072 + q4 * 512, 512)[:, 4 * h4:4 * h4 + 4, :], [("wg", q4)],
                           ceng=next(cvt), cache=winc[:, 4 * h4:4 * h4 + 4, 2072 + q4 * 512:2072 + (q4 + 1) * 512])
            yaT = sbt([128, 4, 512], BF16, "yaT")
            ybT = sbt([128, 4, 512], BF16, "ybT")
            mrg = sbt([128, 8, 512], BF16, "mrg")
            mrg2 = sbt([128, 8, 512], BF16, "mrg2")
            gab = [sbt([128, 512], F32, "ga%d" % i) for i in range(2)]
            gbb = [sbt([128, 512], F32, "gb%d" % i) for i in range(2)]
            xr = [sbt([128, D], F32, "xr%d" % i) for i in range(1)]
            zts = [sbt([128, D], F32, "zt%d" % i) for i in range(2)]
            ht = [sbt([128, D], F32, "ht%d" % i) for i in range(1)]
            hbf = [sbt([128, D], BF16, "hbf%d" % i) for i in range(2)]
            hT = sbt([128, 8, 128], BF16, "hT")
            st6 = sbt([128, 2, 6], F32, "st6")
            mv = sbt([128, 2], F32, "mv")
            rstd = sbt([128, 1], F32, "rstd")
            lg = sbt([128, 32], F32, "lg")
            msk = sbt([128, 32], BF16, "msk")
            slot = sbt([128, 32], F32, "slot")
            tmp32 = sbt([128, 32], F32, "tmp32")
            r8 = sbt([128, 8], F32, "r8")
            nm0 = sbt([128, 1], F32, "nm0")
            ek = sbt([128, 4], F32, "ek")
            esum = sbt([128, 1], F32, "esum")
            destf = sbt([128, 4], F32, "destf")

            mrgs = [mrg, mrg2]

            def stage_mc(T4):
                mrg = mrgs[T4 % 2]
                for (ysrc, yk, yT, yTk) in ((ya, "ya", yaT, "yaT"), (yb, "yb", ybT, "ybT")):
                    for c in range(4):
                        pbt, pbtk = nxt(psT, "psT")
                        for tt in range(4):
                            tr(pbt[:, tt, :], ysrc[:, T4 * 4 + tt, c * 128:(c + 1) * 128], identb[:], [yk, "identb"], [pbtk])
                        act(lambda e, pbt=pbt, c=c, yT=yT: e.copy(yT[:, c, :], pbt[:, 0:4, :].rearrange("p a b -> p (a b)")),
                            [pbtk], [yTk])
                for mc in range(8):
                    ga, gak = nxt(gab, "ga")
                    gb_, gbk = nxt(gbb, "gb")
                    pgA, pgAk = nxt(psS, "psS")
                    for kc in range(8):
                        mm(pgA[:, :], wg[:, kc, mc * 128:(mc + 1) * 128], xT[:, kc, T4 * 512:(T4 + 1) * 512], kc == 0, kc == 7,
                           [("wg", mc // 4)] + xTkeys[T4 * 4:(T4 + 1) * 4], [pgAk])
                    act(lambda e, pgA=pgA, ga=ga, mc=mc: e.activation(out=ga[:], in_=pgA[:, :], func=AF.Sigmoid,
                                                                      bias=bcol[:, 16 + mc:17 + mc], scale=1.0), [pgAk, "bcol"], [gak])
                    pgB, pgBk = nxt(psS, "psS")
                    for kc in range(8):
                        mm(pgB[:, :], wg[:, kc, 1024 + mc * 128:1024 + (mc + 1) * 128], xT[:, kc, T4 * 512:(T4 + 1) * 512],
                           kc == 0, kc == 7, [("wg", 2 + mc // 4)] + xTkeys[T4 * 4:(T4 + 1) * 4], [pgBk])
                    act(lambda e, pgB=pgB, gb_=gb_, mc=mc: e.activation(out=gb_[:], in_=pgB[:, :], func=AF.Sigmoid,
                                                                        bias=bcol[:, 24 + mc:25 + mc], scale=1.0), [pgBk, "bcol"], [gbk])
                    pA, pAk = nxt(psG, "psG")
                    for kc in range(4):
                        mm(pA[:, :], wba[:, kc, mc * 128:(mc + 1) * 128], yaT[:, kc, :], kc == 0, kc == 3, ["wba", "yaT"], [pAk])
                    dve(lambda e, pA=pA, ga=ga: e.tensor_tensor(out=ga[:], in0=pA[:, :], in1=ga[:], op=ALU.mult), [pAk, gak], [gak])
                    pB, pBk = nxt(psG, "psG")
                    for kc in range(4):
                        mm(pB[:, :], wbb[:, kc, mc * 128:(mc + 1) * 128], ybT[:, kc, :], kc == 0, kc == 3, ["wbb", "ybT"], [pBk])
                    dve(lambda e, pB=pB, gb_=gb_: e.tensor_tensor(out=gb_[:], in0=pB[:, :], in1=gb_[:], op=ALU.mult), [pBk, gbk], [gbk])
                    dve(lambda e, mc=mc, ga=ga, gb_=gb_: e.tensor_tensor(out=mrg[:, mc, :], in0=ga[:], in1=gb_[:], op=ALU.add), [gak, gbk], [("mrg", T4 % 2, mc)])
                    yield

            def stage_tt(T4, tt):
                mrg = mrgs[T4 % 2]
                mrgk = [("mrg", T4 % 2, mc) for mc in range(8)]
                tg = sq * NT + T4 * 4 + tt
                r0 = tg * 128
                xrt, xrk = nxt(xr, "xr")
                zt, ztn = nxt(zts, "zt")
                zi = ztn[1]
                dma("sp", xrt[:], x_d[r0:r0 + 128, :], [], [xrk], "xr0")
                for half in range(2):
                    po_, pok_ = nxt(psO, "psO")
                    pov = po_[:, :, :].rearrange("p a b -> p (a b)")
                    for mc in range(8):
                        mm(pov, mrg[:, mc, tt * 128:(tt + 1) * 128], wout[:, mc, half * 512:(half + 1) * 512], mc == 0, mc == 7,
                           mrgk + ["wout"], [pok_])
                    dve(lambda e, pov=pov, xrt=xrt, half=half, zt=zt: e.scalar_tensor_tensor(
                        out=zt[:, half * 512:(half + 1) * 512], in0=xrt[:, half * 512:(half + 1) * 512], scalar=ALPHA, in1=pov,
                        op0=ALU.mult, op1=ALU.add), [pok_, xrk], [("zt", zi, half)])
                    dve(lambda e, half=half, zt=zt: e.bn_stats(st6[:, half, :], zt[:, half * 512:(half + 1) * 512]), [("zt", zi, half)], ["st6"])
                dve(lambda e: e.bn_aggr(mv[:], st6[:]), ["st6"], ["mv"])
                pool(lambda e: e.tensor_scalar(out=rstd[:], in0=mv[:, 1:2], scalar1=LN_EPS, scalar2=None, op0=ALU.add), ["mv"], ["rstd"])
                pool(lambda e: e.tensor_tensor(out=rstd[:], in0=rstd[:], in1=mhalf[:], op=ALU.pow), ["rstd", "mhalf"], ["rstd"])
                htt, htk = nxt(ht, "ht")
                hb_, hbk = nxt(hbf, "hbf")
                dve(lambda e, zt=zt: e.tensor_scalar(out=zt[:], in0=zt[:], scalar1=mv[:, 0:1], scalar2=rstd[:, 0:1],
                                              op0=ALU.subtract, op1=ALU.mult), [("zt", zi, 0), ("zt", zi, 1), "mv", "rstd"], [("zt", zi, 0), ("zt", zi, 1)])
                dve(lambda e, zt=zt: e.tensor_tensor(out=zt[:], in0=zt[:], in1=ln1g[:], op=ALU.mult), [("zt", zi, 0), ("zt", zi, 1), "ln1g"],
                    [("zt", zi, 0), ("zt", zi, 1)])
                dve(lambda e, htt=htt, zt=zt: e.tensor_tensor(out=htt[:], in0=zt[:], in1=ln1b[:], op=ALU.add), [("zt", zi, 0), ("zt", zi, 1), "ln1b"], [htk])
                dma("sp", hres[r0:r0 + 128, :], htt[:], [htk], [("hres", tg)], "hs%d" % (tg % 2))
                if dbg:
                    dma("sp", dbg_h[r0:r0 + 128, :], htt[:], [htk], [], "hd%d" % (tg % 2))
                dve(lambda e, hb_=hb_, zt=zt: e.tensor_tensor(out=hb_[:], in0=zt[:], in1=ln1b[:], op=ALU.add), [("zt", zi, 0), ("zt", zi, 1), "ln1b"], [hbk])
                newb[0] = stage_tt_b(tg, hb_, hbk)

            def stage_tt_b(tg, hb_, hbk):
                pbt, pbtk = nxt(psT, "psT")
                for kc in range(8):
                    tr(pbt[:, kc, :], hb_[:, kc * 128:(kc + 1) * 128], identb[:], [hbk, "identb"], [pbtk])
                act(lambda e, pbt=pbt: e.copy(hT[:], pbt[:]), [pbtk], ["hT"])
                yield
                pl, plk = nxt(psG, "psG")
                for kc in range(8):
                    mm(pl[:, 0:32], hT[:, kc, :], wr[:, kc, :], kc == 0, kc == 7, ["hT", "wr"], [plk])
                dve(lambda e, pl=pl: e.tensor_tensor(out=lg[:], in0=pl[:, 0:32], in1=brt[:], op=ALU.add), [plk, "brt"], ["lg"])
                dve(lambda e: e.max(r8[:], lg[:]), ["lg"], ["r8"])
                dve(lambda e: e.tensor_scalar(out=msk[:], in0=lg[:], scalar1=r8[:, 3:4], scalar2=None, op0=ALU.is_ge), ["lg", "r8"], ["msk"])
                dve(lambda e, tg=tg: e.tensor_scalar(out=gates_all[:, tg, :], in0=r8[:, 0:4], scalar1=r8[:, 0:1], scalar2=None, op0=ALU.subtract),
                    ["r8"], ["gates_all"])
                yield
                pp, ppk = nxt(psG, "psG")
                mm(pp[:, 0:32], uut[:], msk[:], True, True, ["uut", "msk"], [ppk])
                mm(pp[:, 32:64], onesb[:], msk[:], False, True, ["onesb", "msk"], [ppk])
                dve(lambda e, pp=pp: e.tensor_tensor(out=slot[:], in0=pp[:, 0:32], in1=tot[:], op=ALU.add), [ppk, "tot"], ["slot"])
                dve(lambda e, pp=pp: e.tensor_tensor(out=tot[:], in0=pp[:, 32:64], in1=tot[:], op=ALU.add), [ppk, "tot"], ["tot"])
                dve(lambda e: e.scalar_tensor_tensor(out=slot[:], in0=slot[:], scalar=float(CAP - 1), in1=ecap[:], op0=ALU.min, op1=ALU.add),
                    ["slot", "ecap"], ["slot"])
                dve(lambda e: e.memset(destf[:], 0.0), [], ["destf"])
                for k in range(4):
                    dve(lambda e, k=k: e.scalar_tensor_tensor(out=tmp32[:], in0=lg[:], scalar=r8[:, k:k + 1], in1=slot[:],
                                                              op0=ALU.is_equal, op1=ALU.mult, accum_out=destf[:, k:k + 1]),
                        ["lg", "r8", "slot"], ["tmp32", "destf"])
                dve(lambda e, tg=tg: e.tensor_copy(dest_all[:, tg, :], destf[:]), ["destf"], [("dest", tg)])
                for k in range(4):
                    P.add("pool", lambda e, tg=tg, k=k, hb_=hb_: e.indirect_dma_start(
                        out=xg.ap(), out_offset=bass.IndirectOffsetOnAxis(ap=dest_all[:, tg, k:k + 1], axis=0),
                        in_=hb_[:], in_offset=None), reads=[("dest", tg), hbk], writes=["xg"], dma="sc%d" % k)

            gen = stage_mc(0)
            for _ in gen:
                pass
            prevb = None
            newb = [None]
            for T4 in range(4):
                gen = stage_mc(T4 + 1) if T4 + 1 < 4 else iter(())
                for tt in range(4):
                    if prevb is not None:
                        next(prevb, None)
                    stage_tt(T4, tt)
                    if prevb is not None:
                        next(prevb, None)
                    for _ in range(2):
                        next(gen, None)
                    if prevb is not None:
                        for _ in prevb:
                            pass
                    prevb = newb[0]
                for _ in gen:
                    pass
            for _ in prevb:
                pass
            P.barrier()
        cache_ready[0] = True

    es_seq.close()
    slabs = []
    o = 0
    while o < CAP:
        n = min(512, CAP - o)
        slabs.append((o, n))
        o += n
    with ExitStack() as es:
        def sbt(shape, dt, name):
            uid[0] += 1
            return es.enter_context(nc.sbuf_tensor("%s_%d" % (name, uid[0]), list(shape), dt))

        wgu = [sbt([128, 8, 2 * D], BF16, "wgu%d" % i) for i in range(2)]
        wdn = [sbt([128, 8, D], BF16, "wdn%d" % i) for i in range(2)]
        bdb = [sbt([128, D], F32, "bdb%d" % i) for i in range(2)]
        xe = [sbt([128, 4, D], BF16, "xe%d" % i) for i in range(2)]
        xeT = [sbt([128, 8, 512], BF16, "xeT%d" % i) for i in range(2)]
        aT = [sbt([128, 8, 512], BF16, "aT%d" % i) for i in range(2)]
        glu = [sbt([128, 512], F32, "glu%d" % i) for i in range(2)]
        sg = [sbt([128, 512], F32, "sg%d" % i) for i in range(2)]
        lin = [sbt([128, 512], F32, "lin%d" % i) for i in range(2)]
        t1 = [sbt([128, 512], F32, "t1%d" % i) for i in range(2)]
        ye = [sbt([128, D], BF16, "ye%d" % i) for i in range(2)]

        def ld_split(dst, src, wkeys, ceng):
            i = stgi[0] % len(stg)
            stgi[0] += 1
            shp = list(dst.shape)
            n = 1
            for d_ in shp[1:]:
                n *= d_
            assert n <= STGN
            sv = stg[i][0:shp[0], 0:n]
            if len(shp) == 3:
                sv = sv.rearrange("p (a b) -> p a b", b=shp[2])
            sk = ("stg", i)
            dma("sp", sv, src, [], [sk], "sg%d" % i)

            def cv():
                if ceng == "act":
                    P.add("act", lambda e: e.copy(dst, sv), reads=[sk], writes=wkeys)
                else:
                    P.add(ceng, lambda e: e.tensor_copy(dst, sv), reads=[sk], writes=wkeys)
            return cv

        def expert_loads(e_):
            i = e_ % 2
            chunks = []
            for kc in range(8):
                for hf in range(2):
                    chunks.append((wgu[i][:, kc, hf * 1024:(hf + 1) * 1024], wgu_d[e_, kc * 128:(kc + 1) * 128, hf * 1024:(hf + 1) * 1024],
                                   [("wgu", i, kc)]))
            for kc in range(8):
                chunks.append((wdn[i][:, kc, :], wd_d[e_, kc * 128:(kc + 1) * 128, :], [("wdn", i, kc)]))
            fl = []
            state = {"cv": None}

            def step(c):
                prev = state["cv"]
                if c < len(chunks):
                    d_, s_, k_ = chunks[c]
                    state["cv"] = ld_split(d_, s_, k_, "act")
                else:
                    state["cv"] = None
                if prev is not None:
                    prev()

            for c in range(len(chunks) + 1):
                fl.append(lambda c=c: step(c))
            fl.append(lambda: dma("sp", bdb[i][:], bd_d[e_, :].partition_broadcast(128), [], [("bdb", i)], "bdb%d" % i))
            return fl

        for f_ in expert_loads(0):
            f_()
        pend_down = []
        work = [(e_, o, n) for e_ in range(NE) for (o, n) in slabs]

        def prep(idx):
            e_, o, n = work[idx]
            ntt = n // 128
            xet, xek = nxt(xe, "xe")
            row0 = e_ * CAP + o
            dma("pool", xet[:, 0:ntt, :], xg[row0:row0 + n, :].rearrange("(t p) d -> p t d", p=128), ["xg"], [xek],
                "xe%d" % (rot["xe"] % 2))
            xT_, xTk = nxt(xeT, "xeT")
            for tt in range(ntt):
                pbt, pbtk = nxt(psT, "psT")
                for kc in range(8):
                    tr(pbt[:, kc, :], xet[:, tt, kc * 128:(kc + 1) * 128], identb[:], [xek, "identb"], [pbtk])
                dve(lambda e, pbt=pbt, tt=tt, xT_=xT_: e.tensor_copy(xT_[:, :, tt * 128:(tt + 1) * 128], pbt[:]), [pbtk], [xTk])
            return dict(xT_=xT_, xTk=xTk, ntt=ntt, row0=row0)

        preps = {0: prep(0)}
        pend = []
        for idx, (e_, o, n) in enumerate(work):
            i = e_ % 2
            if o == 0:
                pend = expert_loads(e_ + 1) if e_ + 1 < NE else []
            wguk = [("wgu", i, kc) for kc in range(8)]
            pr = preps.pop(idx)
            xT_, xTk, ntt, row0 = pr["xT_"], pr["xTk"], pr["ntt"], pr["row0"]
            at, atk = nxt(aT, "aT")
            for fc in range(8):
                if pend:
                    pend.pop(0)()
                pg, pgk = nxt(psS, "psS")
                for kc in range(8):
                    mm(pg[:, 0:n], wgu[i][:, kc, fc * 128:(fc + 1) * 128], xT_[:, kc, 0:n], kc == 0, kc == 7, [wguk[kc], xTk], [pgk])
                plin, plk = nxt(psG, "psG")
                for kc in range(8):
                    mm(plin[:, 0:n], wgu[i][:, kc, D + fc * 128:D + (fc + 1) * 128], xT_[:, kc, 0:n], kc == 0, kc == 7,
                       [wguk[kc], xTk], [plk])
                if fc == 1 and pend_down:
                    pend_down.pop(0)()
                if fc == 4 and idx + 1 < len(work):
                    preps[idx + 1] = prep(idx + 1)
                g_, gk = nxt(glu, "glu")
                s__, sk = nxt(sg, "sg")
                l_, lk = nxt(lin, "lin")
                t_, tk = nxt(t1, "t1")
                bg = bguT[:, e_ * 16 + fc:e_ * 16 + fc + 1]
                bl = bguT[:, e_ * 16 + 8 + fc:e_ * 16 + 8 + fc + 1]
                dve(lambda e, pg=pg, g_=g_, bg=bg, n=n: e.tensor_scalar(out=g_[:, 0:n], in0=pg[:, 0:n], scalar1=bg, scalar2=7.0,
                                                                       op0=ALU.add, op1=ALU.min), [pgk, "bguT"], [gk])
                act(lambda e, g_=g_, s__=s__, n=n: e.activation(out=s__[:, 0:n], in_=g_[:, 0:n], func=AF.Sigmoid, scale=1.702), [gk], [sk])
                act(lambda e, plin=plin, l_=l_, bl=bl, n=n: e.activation(out=l_[:, 0:n], in_=plin[:, 0:n], func=AF.Identity, bias=bl, scale=1.0),
                    [plk, "bguT"], [lk])
                dve(lambda e, l_=l_, n=n: e.tensor_scalar(out=l_[:, 0:n], in0=l_[:, 0:n], scalar1=7.0, scalar2=-7.0,
                                                         op0=ALU.min, op1=ALU.max), [lk], [lk])
                dve(lambda e, g_=g_, s__=s__, t_=t_, n=n: e.tensor_tensor(out=t_[:, 0:n], in0=g_[:, 0:n], in1=s__[:, 0:n], op=ALU.mult),
                    [gk, sk], [tk])
                dve(lambda e, t_=t_, l_=l_, at=at, fc=fc, n=n: e.scalar_tensor_tensor(out=at[:, fc, 0:n], in0=l_[:, 0:n], scalar=1.0, in1=t_[:, 0:n],
                                                                                     op0=ALU.add, op1=ALU.mult), [tk, lk], [(atk, fc)])

            def down(at=at, atk=atk, ntt=ntt, row0=row0, i=i):
                atks = [(atk, fc) for fc in range(8)]
                for tt in range(ntt):
                    yt, ytk = nxt(ye, "ye")
                    for half in range(2):
                        po_, pok_ = nxt(psO, "psO")
                        pov = po_[:, :, :].rearrange("p a b -> p (a b)")
                        for fc in range(8):
                            mm(pov, at[:, fc, tt * 128:(tt + 1) * 128], wdn[i][:, fc, half * 512:(half + 1) * 512], fc == 0, fc == 7,
                               atks + [("wdn", i, fc)], [pok_])
                        dve(lambda e, pov=pov, yt=yt, half=half, i=i: e.tensor_tensor(
                            out=yt[:, half * 512:(half + 1) * 512], in0=pov, in1=bdb[i][:, half * 512:(half + 1) * 512], op=ALU.add),
                            [pok_, ("bdb", i)], [(ytk, half)])
                    r0 = row0 + tt * 128
                    dma("pool", yg[r0:r0 + 128, :], yt[:], [(ytk, 0), (ytk, 1)], ["yg"], "ys%d" % (rot["ye"] % 2))

            pend_down.append(down)
            if o + n >= CAP:
                while pend:
                    pend.pop(0)()
        while pend_down:
            pend_down.pop(0)()
        P.barrier()

    with ExitStack() as es:
        def sbt(shape, dt, name):
            uid[0] += 1
            return es.enter_context(nc.sbuf_tensor("%s_%d" % (name, uid[0]), list(shape), dt))

        ln2g = sbt([128, D], F32, "ln2g")
        ln2b = sbt([128, D], F32, "ln2b")
        dma("sp", ln2g[:], ln2g_d.ap().partition_broadcast(128), [], ["ln2g"], "c0")
        dma("sp", ln2b[:], ln2b_d.ap().partition_broadcast(128), [], ["ln2b"], "c1")
        gsum = sbt([128, NTT], F32, "gsum")
        act(lambda e: e.activation(out=gates_all[:], in_=gates_all[:], func=AF.Exp), ["gates_all"], ["gates_all"])
        dve(lambda e: e.tensor_reduce(out=gsum[:], in_=gates_all[:], axis=mybir.AxisListType.X, op=ALU.add), ["gates_all"], ["gsum"])
        dve(lambda e: e.reciprocal(gsum[:], gsum[:]), ["gsum"], ["gsum"])
        dve(lambda e: e.tensor_tensor(out=gates_all[:], in0=gates_all[:], in1=gsum[:, :].unsqueeze(2).to_broadcast([128, NTT, 4]), op=ALU.mult),
            ["gates_all", "gsum"], ["gates_all"])
        NBC = 3
        hr = [sbt([128, D], F32, "hr%d" % i) for i in range(NBC)]
        yk4 = [[sbt([128, D], BF16, "yk%d_%d" % (i, k)) for k in range(4)] for i in range(NBC)]
        zz = [sbt([128, D], F32, "zz%d" % i) for i in range(NBC)]
        oo = [sbt([128, D], F32, "oo%d" % i) for i in range(2)]
        st6b = sbt([128, 2, 6], F32, "st6b")
        mvb = sbt([128, 2], F32, "mvb")
        rstdb = sbt([128, 1], F32, "rstdb")
        nmrb = sbt([128, 1], F32, "nmrb")

        def c_loads(tg):
            i = tg % NBC
            r0 = tg * 128
            dma("sp", hr[i][:], hres[r0:r0 + 128, :], [("hres", tg)], [("hr", i)], "hr%d" % i)
            for k in range(4):
                P.add("pool", lambda e, tg=tg, k=k, i=i: e.indirect_dma_start(
                    out=yk4[i][k][:], out_offset=None, in_=yg.ap(),
                    in_offset=bass.IndirectOffsetOnAxis(ap=dest_all[:, tg, k:k + 1], axis=0)),
                    reads=["yg"], writes=[("yk", i, k)], dma="gk%d%d" % (i, k))

        dgs = [[sbt([128, 128], BF16, "dg%d_%d" % (i, k)) for k in range(4)] for i in range(2)]

        st6c = [sbt([128, 2, 6], F32, "st6c%d" % i) for i in range(2)]
        mvc = [sbt([128, 2], F32, "mvc%d" % i) for i in range(2)]
        rstdc = [sbt([128, 1], F32, "rstdc%d" % i) for i in range(2)]
        nmrc = [sbt([128, 1], F32, "nmrc%d" % i) for i in range(2)]

        def c_compute1(tg):
            i = tg % NBC
            j = tg % 2
            z = zz[i]
            zk = ("zz", i)
            for k in range(4):
                act(lambda e, j=j, k=k, tg=tg: e.activation(out=dgs[j][k][:], in_=identb[:], func=AF.Copy, scale=gates_all[:, tg, k:k + 1]),
                    ["identb", "gates_all"], [("dg", j, k)])
            for half in range(2):
                po_, pok_ = nxt(psS, "psS")
                for k in range(4):
                    mm(po_[:, :], dgs[j][k][:], yk4[i][k][:, half * 512:(half + 1) * 512], k == 0, k == 3, [("dg", j, k), ("yk", i, k)], [pok_])
                dve(lambda e, z=z, i=i, half=half, po_=po_: e.scalar_tensor_tensor(
                    out=z[:, half * 512:(half + 1) * 512], in0=hr[i][:, half * 512:(half + 1) * 512], scalar=ALPHA, in1=po_[:, :],
                    op0=ALU.mult, op1=ALU.add), [pok_, ("hr", i)], [(zk, half)])
                dve(lambda e, z=z, half=half, j=j: e.bn_stats(st6c[j][:, half, :], z[:, half * 512:(half + 1) * 512]), [(zk, half)], [("st6c", j)])
            dve(lambda e, j=j: e.bn_aggr(mvc[j][:], st6c[j][:]), [("st6c", j)], [("mvc", j)])
            act(lambda e, j=j: e.activation(out=rstdc[j][:], in_=mvc[j][:, 1:2], func=AF.Ln, bias=epsb[:, 0:1], scale=1.0), [("mvc", j), "epsb"], [("rstdc", j)])
            act(lambda e, j=j: e.activation(out=rstdc[j][:], in_=rstdc[j][:], func=AF.Exp, scale=-0.5), [("rstdc", j)], [("rstdc", j)])
            dve(lambda e, j=j: e.scalar_tensor_tensor(out=nmrc[j][:], in0=mvc[j][:, 0:1], scalar=-1.0, in1=rstdc[j][:], op0=ALU.mult, op1=ALU.mult),
                [("mvc", j), ("rstdc", j)], [("nmrc", j)])

        def c_compute2(tg):
            i = tg % NBC
            j = tg % 2
            r0 = tg * 128
            z = zz[i]
            zk = ("zz", i)
            zks = [(zk, 0), (zk, 1)]
            act(lambda e, z=z, j=j: e.activation(out=z[:], in_=z[:], func=AF.Identity, bias=nmrc[j][:, 0:1], scale=rstdc[j][:, 0:1]),
                zks + [("nmrc", j), ("rstdc", j)], zks)
            dve(lambda e, z=z: e.tensor_tensor(out=z[:], in0=z[:], in1=ln2g[:], op=ALU.mult), zks + ["ln2g"], zks)
            dve(lambda e, z=z, j=j: e.tensor_tensor(out=oo[j][:], in0=z[:], in1=ln2b[:], op=ALU.add), zks + ["ln2b"], [("oo", j)])
            dma("sp", out_d[r0:r0 + 128, :], oo[j][:], [("oo", j)], [], "os%d" % j)

        c_loads(0)
        if NTT > 1:
            c_loads(1)
        for tg in range(NTT):
            if tg + 2 < NTT:
                c_loads(tg + 2)
            c_compute1(tg)
            if tg >= 1:
                c_compute2(tg - 1)
        c_compute2(NTT - 1)
    P.emit()
    return nc


def _prep_inputs(inputs, NSEQ, CAP, ncores):
    f32 = lambda a: np.ascontiguousarray(np.asarray(a, dtype=np.float32))
    x = f32(inputs["x"])
    shared = {}
    for k in ("w_in", "b_in", "attn_sinks", "cmp_pos_k", "cmp_w1_k", "cmp_w2_k", "cmp_pos_v", "cmp_w1_v", "cmp_w2_v",
              "w_branch_a", "w_branch_b", "w_out", "ln1_g", "ln1_b", "w_router", "b_router", "w_gate_up", "b_gate_up",
              "w_down", "b_down", "ln2_g", "ln2_b"):
        shared[k] = f32(inputs[k])[0]
    shared["rel_bias"] = f32(inputs["rel_bias"])
    shared.update(make_consts(CAP))
    in_maps = []
    for c in range(ncores):
        m = dict(shared)
        m["x"] = np.ascontiguousarray(x[c * NSEQ:(c + 1) * NSEQ].reshape(NSEQ * S, D))
        in_maps.append(m)
    return in_maps


def kernel(**inputs):
    NSEQ, CAP, ncores = 4, 1280, 8
    nc = build(NSEQ, CAP)
    in_maps = _prep_inputs(inputs, NSEQ, CAP, ncores)
    res = run_bass_kernel_spmd(nc, in_maps, core_ids=list(range(ncores)))
    out = np.stack([np.asarray(r["out"]).reshape(NSEQ, S, D) for r in res.results], axis=0)
    return out.reshape(ncores * NSEQ, S, D).astype(np.float32)
```

```python
import math
from collections import defaultdict
from contextlib import ExitStack
import numpy as np
import ml_dtypes
import concourse.bass as bass
import concourse.mybir as mybir
from concourse.bass_utils import run_bass_kernel_spmd

F32 = mybir.dt.float32
BF16 = mybir.dt.bfloat16
I32 = mybir.dt.int32
U32 = mybir.dt.uint32
AF = mybir.ActivationFunctionType
ALU = mybir.AluOpType

S = 2048
D = 1024
NT = 16
NEGM = -30000.0
ALPHA = 2.0 ** 0.25
LN_EPS = 1e-5
NE = 32


class _Op:
    __slots__ = ("eng", "fn", "waits", "signal", "track", "seq", "clock", "ninst", "isdma", "val")

    def __init__(self, eng, fn):
        self.eng = eng
        self.fn = fn
        self.waits = []
        self.signal = False
        self.ninst = 1
        self.isdma = False
        self.val = None


class Prog:
    ENGS = ("pe", "act", "dve", "pool", "sp")

    def __init__(self, nc):
        self.nc = nc
        self.ops = {e: [] for e in self.ENGS}
        self.known = {e: {} for e in self.ENGS}
        self.lastw = {}
        self.readers = defaultdict(list)
        self.track_ops = defaultdict(list)

    def _dep(self, op, d, kind):
        if d is None:
            return
        if d.track == op.eng and not op.isdma:
            if op.eng == "pe" or kind != "raw":
                return
        kn = self.known[op.eng]
        if kn.get(d.track, 0) >= d.seq:
            return
        op.waits.append(d)
        d.signal = True
        for t, s in d.clock.items():
            if kn.get(t, 0) < s:
                kn[t] = s

    def add(self, eng, fn, reads=(), writes=(), dma=None, ninst=1):
        op = _Op(eng, fn)
        op.ninst = ninst
        op.isdma = dma is not None
        op.track = ("dma:" + dma) if dma else eng
        for k in reads:
            self._dep(op, self.lastw.get(k), "raw")
        for k in writes:
            self._dep(op, self.lastw.get(k), "waw")
            for r in self.readers.get(k, ()):
                self._dep(op, r, "war")
        tl = self.track_ops[op.track]
        if op.isdma and tl:
            self._dep(op, tl[-1], "raw")
        op.seq = len(tl) + 1
        tl.append(op)
        ck = dict(self.known[op.eng])
        ck[op.track] = op.seq
        op.clock = ck
        for k in reads:
            self.readers[k].append(op)
        for k in writes:
            self.lastw[k] = op
            self.readers[k] = []
        self.ops[eng].append(op)
        return op

    def barrier(self):
        lasts = [tl[-1] for tl in self.track_ops.values() if tl]
        for e in self.ENGS:
            op = _Op(e, None)
            op.track = None
            kn = self.known[e]
            for d in lasts:
                if d.track == e and e == "pe":
                    continue
                if kn.get(d.track, 0) >= d.seq:
                    continue
                op.waits.append(d)
                d.signal = True
            self.ops[e].append(op)
        full = {d.track: d.seq for d in lasts}
        for e in self.ENGS:
            self.known[e] = dict(full)
        self.lastw = {}
        self.readers = defaultdict(list)

    def emit(self):
        nc = self.nc
        sems = {}
        for t, tl in self.track_ops.items():
            sems[t] = nc.alloc_semaphore("s_" + t.replace(":", "_"))
            c = 0
            for op in tl:
                if op.isdma:
                    c += 16 * op.ninst
                elif op.signal:
                    c += 1
                op.val = c
        engobj = {"pe": "tensor", "act": "scalar", "dve": "vector", "pool": "gpsimd", "sp": "sync"}
        lasts = [tl[-1] for tl in self.track_ops.values() if tl]
        with nc.Block() as block:
            for ename in self.ENGS:
                ops = self.ops[ename]

                def body(e, ops=ops, ename=ename):
                    for op in ops:
                        for d in op.waits:
                            e.wait_ge(sems[d.track], d.val)
                        if op.fn is None:
                            continue
                        r = op.fn(e)
                        if op.isdma:
                            rl = r if isinstance(r, (list, tuple)) else [r]
                            assert len(rl) == op.ninst
                            for ins in rl:
                                ins.then_inc(sems[op.track], 16)
                        elif op.signal:
                            ins = r[-1] if isinstance(r, (list, tuple)) else r
                            ins.then_inc(sems[op.track], 1)
                    if ename == "sp":
                        for d in lasts:
                            if d.val:
                                e.wait_ge(sems[d.track], d.val)

                getattr(block, engobj[ename])(body)


def _t5_bucket(rel):
    n = np.maximum(rel, 0)
    nf = np.maximum(n, 1).astype(np.float32)
    large = 16 + (np.log(nf / np.float32(16)) / np.float32(math.log(8.0)) * np.float32(16)).astype(np.int32)
    large = np.minimum(large, 31)
    return np.where(n < 16, n, large)


def _onehot(cls, J):
    oh = np.zeros((33, J), np.float32)
    oh[cls, np.arange(len(cls))] = 1.0
    return oh


def make_consts(CAP):
    bf = ml_dtypes.bfloat16
    c = {}
    c["c_identf"] = np.eye(128, dtype=np.float32)
    c["c_identb"] = np.eye(128, dtype=np.float32).astype(bf)
    c["c_antib"] = np.eye(128, dtype=np.float32)[::-1].copy().astype(bf)
    i = np.arange(384)
    rel = i - 127
    c["c_ohA"] = _onehot(np.where((rel < 0) | (rel >= 128), 32, _t5_bucket(rel)), 384)
    i = np.arange(768)
    rel = i - 127
    c["c_ohW"] = _onehot(np.where((rel < 0) | (rel >= 512), 32, _t5_bucket(rel)), 768)
    i = np.arange(640)
    rel = i + 1
    c["c_ohS"] = _onehot(_t5_bucket(rel), 640)
    cc = np.arange(127)[:, None]
    t = np.arange(S)[None, :]
    c["c_maskC"] = np.where(cc * 16 + 31 <= t, 0.0, NEGM).astype(bf)
    cs = np.arange(127)[:, None] * 16
    ss = np.arange(32)[None, :] * 64
    ov = np.clip(np.minimum(cs + 32, ss + 64) - np.maximum(cs, ss), 0, None)
    c["c_mcs"] = (ov / 32.0).astype(bf)
    tt = np.arange(S)
    cur = (tt // 64)[:, None]
    blk = np.arange(32)[None, :]
    forced = (blk == 0) | ((blk <= cur) & (blk > cur - 2))
    future = blk > cur
    nf = (~forced & ~future).astype(np.float32)
    addc = np.where(future, -100.0, np.where(forced, 100.0, 0.0)).astype(np.float32)

    def lay(a):
        a = a.reshape(16, 128, 32).transpose(1, 0, 2)
        return np.ascontiguousarray(np.repeat(a[:, :, None, :], 2, axis=2)).astype(np.float32).astype(bf)

    c["c_nf"] = lay(nf)
    c["c_addc"] = lay(addc)
    ex = np.zeros((32, 16, 128), np.float32)
    for kb in range(16):
        for m in range(128):
            ex[2 * kb + m // 64, kb, m] = 30000.0
    c["c_ex"] = ex.astype(bf)
    c["c_uut"] = np.triu(np.ones((128, 128), np.float32), 1).astype(bf)
    c["c_ones"] = np.ones((128, 128), np.float32).astype(bf)
    c["c_ecap"] = np.tile((np.arange(32, dtype=np.float32) * CAP)[None, :], (128, 1))
    return c


CONST_DT = {"c_identf": F32, "c_identb": BF16, "c_antib": BF16, "c_ohA": F32, "c_ohW": F32, "c_ohS": F32,
            "c_maskC": BF16, "c_mcs": BF16, "c_nf": BF16, "c_addc": BF16, "c_ex": BF16, "c_uut": BF16,
            "c_ones": BF16, "c_ecap": F32}


def build(NSEQ, CAP, dbg=False):
    nc = bass.Bass("TRN2", target_bir_lowering=False)
    P = Prog(nc)
    NTOK = NSEQ * S
    NTT = NTOK // 128
    consts = make_consts(CAP)

    def din(name, shape, dt=F32):
        return nc.dram_tensor(name, list(shape), dt, kind="ExternalInput")

    x_d = din("x", [NTOK, D])
    w_in = din("w_in", [D, 4120])
    b_in = din("b_in", [4120])
    rel_bias = din("rel_bias", [32, 16])
    sinks_d = din("attn_sinks", [8])
    pos_k = din("cmp_pos_k", [32, 64])
    w1_k = din("cmp_w1_k", [2048, 128])
    w2_k = din("cmp_w2_k", [128, 64])
    pos_v = din("cmp_pos_v", [32, 64])
    w1_v = din("cmp_w1_v", [2048, 128])
    w2_v = din("cmp_w2_v", [128, 64])
    wba_d = din("w_branch_a", [512, D])
    wbb_d = din("w_branch_b", [512, D])
    wout_d = din("w_out", [D, D])
    ln1g_d = din("ln1_g", [D])
    ln1b_d = din("ln1_b", [D])
    wr_d = din("w_router", [D, NE])
    br_d = din("b_router", [NE])
    wgu_d = din("w_gate_up", [NE, D, 2 * D])
    bgu_d = din("b_gate_up", [NE, 2 * D])
    wd_d = din("w_down", [NE, D, D])
    bd_d = din("b_down", [NE, D])
    ln2g_d = din("ln2_g", [D])
    ln2b_d = din("ln2_b", [D])
    cd = {k: din(k, v.shape, CONST_DT[k]) for k, v in consts.items()}
    out_d = nc.dram_tensor("out", [NTOK, D], F32, kind="ExternalOutput")
    if dbg:
        dbg_h = nc.dram_tensor("dbg_h", [NTOK, D], F32, kind="ExternalOutput")

    winc = nc.dram_tensor("winc", [128, 8, 4120], BF16, kind="Internal")
    w1c = nc.dram_tensor("w1c", [2, 128, 32, 128], BF16, kind="Internal")
    wbac = nc.dram_tensor("wbac", [128, 4, D], BF16, kind="Internal")
    wbbc = nc.dram_tensor("wbbc", [128, 4, D], BF16, kind="Internal")
    woutc = nc.dram_tensor("woutc", [128, 8, D], BF16, kind="Internal")
    gdA = nc.dram_tensor("gdA", [8, 384], F32, kind="Internal")
    gdW = nc.dram_tensor("gdW", [8, 768], F32, kind="Internal")
    gdS = nc.dram_tensor("gdS", [8, 640], F32, kind="Internal")
    hres = nc.dram_tensor("hres", [NTOK, D], F32, kind="Internal")
    xg = nc.dram_tensor("xg", [NE * CAP, D], BF16, kind="Internal")
    yg = nc.dram_tensor("yg", [NE * CAP, D], BF16, kind="Internal")

    uid = [0]

    def sbp(shape, dt, name=None):
        uid[0] += 1
        return nc.alloc_sbuf_tensor("%s_%d" % (name or "t", uid[0]), list(shape), dt)

    psS = [nc.alloc_psum_tensor("psS%d" % i, [128, 512], F32) for i in range(2)]
    psO = [nc.alloc_psum_tensor("psO%d" % i, [128, 4, 128], F32) for i in range(2)]
    psG = [nc.alloc_psum_tensor("psG%d" % i, [128, 512], F32) for i in range(2)]
    psT = [nc.alloc_psum_tensor("psT%d" % i, [128, 8, 128], BF16) for i in range(2)]
    rot = defaultdict(int)

    def nxt(lst, name):
        i = rot[name] % len(lst)
        rot[name] += 1
        return lst[i], (name, i)

    def mm(out, lhsT, rhs, start, stop, reads, writes):
        P.add("pe", lambda e: e.matmul(out, lhsT=lhsT, rhs=rhs, start=start, stop=stop, skip_group_check=True),
              reads=reads, writes=writes)

    def tr(out, in_, ident, reads, writes):
        P.add("pe", lambda e: e.transpose(out, in_, ident), reads=reads, writes=writes)

    def dma(eng, out, in_, reads, writes, sem):
        P.add(eng, lambda e: e.dma_start(out=out, in_=in_), reads=reads, writes=writes, dma=sem)

    def dve(fn, reads, writes):
        P.add("dve", fn, reads=reads, writes=writes)

    def act(fn, reads, writes):
        P.add("act", fn, reads=reads, writes=writes)

    def pool(fn, reads, writes):
        P.add("pool", fn, reads=reads, writes=writes)

    cst_i = [0]

    def ldc(out, in_, key, eng="sp"):
        cst_i[0] += 1
        dma(eng, out, in_, [], [key], "c%d" % (cst_i[0] % 4) if eng == "sp" else "cp%d" % (cst_i[0] % 2))

    STGN = 1024
    stg = [sbp([128, STGN], F32, "stg%d" % i) for i in range(2)]
    stgi = [0]

    cache_ready = [False]
    cli = [0]

    def ldcast(dst, src, wkeys, p0=0, ceng="pool", deng="sp", cache=None):
        if cache is not None and cache_ready[0]:
            cli[0] += 1
            dma(deng, dst, cache, ["wcache"], wkeys, "cl%d" % (cli[0] % 4))
            return
        shp = list(dst.shape)
        n = 1
        for d_ in shp[1:]:
            n *= d_
        if n > STGN:
            hsz = shp[1] // 2
            assert hsz * 2 == shp[1]
            ix = (slice(None), slice(0, hsz)) + (slice(None),) * (len(shp) - 2)
            iy = (slice(None), slice(hsz, shp[1])) + (slice(None),) * (len(shp) - 2)
            ldcast(dst[ix], src[ix], wkeys, p0, ceng, deng, cache[ix] if cache is not None else None)
            ldcast(dst[iy], src[iy], wkeys, p0, ceng, deng, cache[iy] if cache is not None else None)
            return
        i = stgi[0] % len(stg)
        stgi[0] += 1
        sv = stg[i][p0:p0 + shp[0], 0:n]
        if len(shp) == 3:
            sv = sv.rearrange("p (a b) -> p a b", b=shp[2])
        sk = ("stg", i)
        dma(deng, sv, src, [], [sk], "sg%d" % i)
        if ceng == "act":
            P.add("act", lambda e: e.copy(dst, sv), reads=[sk], writes=wkeys)
        else:
            P.add(ceng, lambda e: e.tensor_copy(dst, sv), reads=[sk], writes=wkeys)
        if cache is not None:
            cli[0] += 1
            dma("sp", cache, dst, wkeys, ["wcache"], "cs%d" % (cli[0] % 2))

    identf = sbp([128, 128], F32, "identf"); ldc(identf[:], cd["c_identf"].ap(), "identf")
    identb = sbp([128, 128], BF16, "identb"); ldc(identb[:], cd["c_identb"].ap(), "identb")
    antib = sbp([128, 128], BF16, "antib"); ldc(antib[:], cd["c_antib"].ap(), "antib")
    uut = sbp([128, 128], BF16, "uut"); ldc(uut[:], cd["c_uut"].ap(), "uut")
    onesb = sbp([128, 128], BF16, "onesb"); ldc(onesb[:], cd["c_ones"].ap(), "onesb")
    ecap = sbp([128, 32], F32, "ecap"); ldc(ecap[:], cd["c_ecap"].ap(), "ecap")
    c31bc = sbp([128, 16], F32, "c31bc"); ldc(c31bc[:], rel_bias[31:32, :].partition_broadcast(128), "c31bc")
    esink = sbp([128, 8], F32, "esink"); ldc(esink[:], sinks_d.ap().partition_broadcast(128), "esink")
    act(lambda e: e.activation(out=esink[:], in_=esink[:], func=AF.Exp), ["esink"], ["esink"])
    btok = sbp([128, 408], F32, "btok")
    for j, c0 in enumerate((640, 1664, 1920)):
        ldc(btok[:, j * 128:(j + 1) * 128], b_in[c0:c0 + 128].partition_broadcast(128), "btok")
    ldc(btok[:, 384:408], b_in[2048:2072].partition_broadcast(128), "btok")
    brt = sbp([128, 32], F32, "brt"); ldc(brt[:], br_d.ap().partition_broadcast(128), "brt")
    wr = sbp([128, 8, 32], BF16, "wr")
    ldcast(wr[:], wr_d.ap().rearrange("(kc p) n -> p kc n", p=128), ["wr"])
    w2kd = sbp([128, 128], BF16, "w2kd")
    ldcast(w2kd[:, 0:64], w2_k.ap(), ["w2kd"])
    ldcast(w2kd[:, 64:128], w2_k.ap(), ["w2kd"])
    w2vb = sbp([128, 64], BF16, "w2vb"); ldcast(w2vb[:], w2_v.ap(), ["w2vb"])
    gates_all = sbp([128, NTT, 4], F32, "gates_all")
    dest_all = sbp([128, NTT, 4], I32, "dest_all")
    tot = sbp([128, 32], F32, "tot")
    dve(lambda e: e.memset(tot[:], 0.0), [], ["tot"])
    bcol = sbp([128, 32], F32, "bcol")
    posT = sbp([64, 2, 32], BF16, "posT")
    bguT = sbp([128, NE * 16], F32, "bguT")
    mhalf = sbp([128, 1], F32, "mhalf")
    dve(lambda e: e.memset(mhalf[:], -0.5), [], ["mhalf"])
    epsb = sbp([128, 1], F32, "epsb")
    dve(lambda e: e.memset(epsb[:], LN_EPS), [], ["epsb"])
    btd = {"A": nc.dram_tensor("btdA", [128, 8, 256], BF16, kind="Internal"),
           "W": nc.dram_tensor("btdW", [128, 8, 640], BF16, kind="Internal"),
           "S": nc.dram_tensor("btdS", [128, 8, 512], BF16, kind="Internal")}
    with ExitStack() as es:
        def sbt(shape, dt, name):
            uid[0] += 1
            return es.enter_context(nc.sbuf_tensor("%s_%d" % (name, uid[0]), list(shape), dt))

        zrow = sbt([128, D], BF16, "zrow")
        dve(lambda e: e.memset(zrow[:], 0.0), [], ["zrow"])
        for e_ in range(NE):
            dma("sp", xg[e_ * CAP:(e_ + 1) * CAP, :].rearrange("(t p) d -> p t d", p=128),
                zrow[:, :].unsqueeze(1).to_broadcast([128, CAP // 128, D]), ["zrow"], ["xg"], "zx%d" % (e_ % 2))

        relext = sbt([33, 16], F32, "relext")
        BTA = sbt([128, 8, 256], BF16, "BTA")
        BTW = sbt([128, 8, 640], BF16, "BTW")
        BTS = sbt([128, 8, 512], BF16, "BTS")
        ldc(relext[0:32, :], rel_bias.ap(), "relext")
        dve(lambda e: e.memset(relext[32:33, :], NEGM), [], ["relext32"])
        for (vn, ohd, J, W, col0, gd, BT) in (("A", cd["c_ohA"], 384, 256, 0, gdA, BTA),
                                              ("W", cd["c_ohW"], 768, 640, 8, gdW, BTW),
                                              ("S", cd["c_ohS"], 640, 512, 8, gdS, BTS)):
            oh = sbt([33, J], F32, "oh" + vn)
            ldc(oh[:], ohd.ap(), "oh" + vn)
            Fv = sbt([8, J], F32, "Fv" + vn)
            for c0 in range(0, J, 512):
                n = min(512, J - c0)
                pb, pk = nxt(psG, "psG")
                mm(pb[0:8, 0:n], relext[0:33, col0:col0 + 8], oh[0:33, c0:c0 + n], True, True,
                   ["relext", "relext32", "oh" + vn], [pk])
                dve(lambda e, pb=pb, c0=c0, n=n, Fv=Fv: e.tensor_copy(Fv[:, c0:c0 + n], pb[0:8, 0:n]), [pk], ["Fv" + vn])
            dma("sp", gd.ap(), Fv[:], ["Fv" + vn], ["gd" + vn], "gd")
            for h in range(8):
                U = sbt([128, W], F32, "U%s%d" % (vn, h))
                Ub = sbt([128, W], BF16, "Ub%s%d" % (vn, h))
                uk = "U%s%d" % (vn, h)
                dma("sp", U[:], bass.AP(gd, h * J, [[1, 128], [1, W]]), ["gd" + vn], [uk], "u%d" % (h % 2))
                dve(lambda e, U=U, Ub=Ub: e.tensor_copy(Ub[:], U[:]), [uk], [uk + "b"])
                for c0 in range(0, W, 512):
                    n = min(512, W - c0)
                    pb, pk = nxt(psG, "psG")
                    mm(pb[:, 0:n], antib[:], Ub[:, c0:c0 + n], True, True, ["antib", uk + "b"], [pk])
                    dve(lambda e, pb=pb, c0=c0, n=n, BT=BT, h=h: e.tensor_copy(BT[:, h, c0:c0 + n], pb[:, 0:n]),
                        [pk], ["BT" + vn])
            dma("sp", btd[vn].ap(), BT[:], ["BT" + vn], ["btd" + vn], "gd")
        brow = sbt([32, 128], F32, "brow")
        ldc(brow[0:16, :], b_in[0:2048].rearrange("(c p) -> c p", p=128), "brow")
        ldc(brow[16:32, :], b_in[2072:4120].rearrange("(c p) -> c p", p=128), "brow")
        pb, pk = nxt(psG, "psG")
        tr(pb[:, 0:32], brow[0:32, :], identf[0:32, 0:32], ["brow", "identf"], [pk])
        dve(lambda e, pb=pb: e.tensor_copy(bcol[:], pb[:, 0:32]), [pk], ["bcol"])
        for kv, pd in enumerate((pos_k, pos_v)):
            pr = sbt([32, 64], F32, "posr%d" % kv)
            ldc(pr[:], pd.ap(), "posr%d" % kv)
            pb, pk = nxt(psG, "psG")
            tr(pb[0:64, 0:32], pr[0:32, :], identf[0:32, 0:32], ["posr%d" % kv, "identf"], [pk])
            dve(lambda e, pb=pb, kv=kv: e.tensor_copy(posT[:, kv, :], pb[0:64, 0:32]), [pk], ["posT"])
        bgv = bgu_d.ap().rearrange("e (c p) -> (e c) p", p=128)
        for r in range(4):
            bt_ = sbt([128, 128], F32, "bgr%d" % r)
            ldc(bt_[:], bgv[r * 128:(r + 1) * 128, :], "bgr%d" % r)
            pb, pk = nxt(psG, "psG")
            tr(pb[:, 0:128], bt_[:], identf[:], ["bgr%d" % r, "identf"], [pk])
            dve(lambda e, pb=pb, r=r: e.tensor_copy(bguT[:, r * 128:(r + 1) * 128], pb[:, 0:128]), [pk], ["bguT"])
        P.barrier()

    es_seq = ExitStack()

    def sbq(shape, dt, name):
        uid[0] += 1
        return es_seq.enter_context(nc.sbuf_tensor("%s_%d" % (name, uid[0]), list(shape), dt))

    xT = sbq([128, 8, S], BF16, "xT")
    ya = sbq([128, NT, 512], BF16, "ya")
    yb = sbq([128, NT, 512], BF16, "yb")
    wcols = {"qA": 0, "kA": 512, "vA": 640, "qB": 768, "kBc": 1280, "vBc": 1408, "kBs": 1536, "vBs": 1664,
             "kBw": 1792, "vBw": 1920, "ng": 2048, "mg": 2072}

    def w_in_view(c0, n):
        return w_in[:, c0:c0 + n].rearrange("(kc p) n -> p kc n", p=128)

    for sq in range(NSEQ):
        tok0 = sq * S
        with ExitStack() as es:
            def sbt(shape, dt, name):
                uid[0] += 1
                return es.enter_context(nc.sbuf_tensor("%s_%d" % (name, uid[0]), list(shape), dt))

            xld = [sbt([128, D], BF16, "xld%d" % i) for i in range(2)]
            maskC = sbt([128, S], BF16, "maskC"); ldc(maskC[0:127, :], cd["c_maskC"].ap(), "maskC")
            nfm = sbt([128, 16, 2, 32], BF16, "nfm"); ldc(nfm[:], cd["c_nf"].ap(), "nfm")
            addc = sbt([128, 16, 2, 32], BF16, "addc"); ldc(addc[:], cd["c_addc"].ap(), "addc")
            exm = sbt([128, 16, 128], BF16, "exm")
            pool(lambda e: e.memset(exm[:], 0.0), [], ["exm"])
            ldc(exm[0:32, :, :], cd["c_ex"].ap(), "exm")
            BTA = sbt([128, 8, 256], BF16, "BTA"); ldc(BTA[:], btd["A"].ap(), "BTA")
            BTW = sbt([128, 8, 640], BF16, "BTW"); ldc(BTW[:], btd["W"].ap(), "BTW")
            wst = [sbt([128, 8, 256], BF16, "wst%d" % i) for i in range(2)]
            QT = sbt([128, 4, S], BF16, "QT")
            K1 = sbt([128, 2, 2, S], BF16, "K1")
            K2 = sbt([128, 2, 2, S], BF16, "K2")
            dve(lambda e: e.memset(K1[:], 0.0), [], ["K1"])
            dve(lambda e: e.memset(K2[:], 0.0), [], ["K2"])
            V1 = sbt([128, NT, 2, 65], BF16, "V1")
            V2 = sbt([128, NT, 2, 65], BF16, "V2")
            Gt = sbt([128, NT, 24], F32, "Gt")
            KcT = sbt([128, 2, 2, 128], BF16, "KcT")
            dve(lambda e: e.memset(KcT[:], 0.0), [], ["KcT"])
            VcE = sbt([128, 2, 97], BF16, "VcE")
            acc = sbt([128, 4, 512], F32, "acc")
            KC = acc[:, :, :].rearrange("p a b -> p (a b)").bitcast(BF16).rearrange("p (j s) -> p j s", j=2)
            pTs = [sbt([128, 512], BF16, "pT%d" % i) for i in range(5)]
            imp = sbt([128, 4, 2, 32], F32, "imp")
            impm = sbt([128, 4, 2, 32], F32, "impm")
            selb = sbt([128, 4, 2, 32], BF16, "selb")
            selT = sbt([128, 2, 512], BF16, "selT")
            pool(lambda e: e.memset(selT[:], 0.0), [], ["selT"])
            m8 = sbt([128, 8, 8], F32, "m8")
            den = sbt([128, 4], F32, "den")
            rec = sbt([128, 4], F32, "rec")
            recg = sbt([128, 4], F32, "recg")
            gx = sbt([128, 128], F32, "gx")
            gt_ = sbt([128, 128], F32, "gt")
            gs = sbt([128, 128], F32, "gs")
            hact = sbt([128, 128], BF16, "hact")
            posb = sbt([128, 2], F32, "posb")

            for t in range(NT):
                xl = xld[t % 2]
                xk = ("xld", t % 2)
                ldcast(xl[:], x_d[tok0 + t * 128: tok0 + (t + 1) * 128, :], [xk], ceng="act")
                pb, pk = nxt(psT, "psT")
                for kc in range(8):
                    tr(pb[:, kc, :], xl[:, kc * 128:(kc + 1) * 128], identb[:], [xk, "identb"], [pk])
                dve(lambda e, pb=pb, t=t: e.tensor_copy(xT[:, :, t * 128:(t + 1) * 128], pb[:]), [pk], [("xT", t)])
            xTkeys = [("xT", t) for t in range(NT)]

            wsti = [0]

            def load_w(c0, n, dup64=False):
                i = wsti[0] % 2
                wsti[0] += 1
                wt = wst[i]
                wk = ("wst", i)
                if dup64:
                    for j in range(4):
                        src = c0 + (j // 2) * 64
                        ldcast(wt[:, :, j * 64:(j + 1) * 64], w_in_view(src, 64), [wk], cache=winc[:, :, src:src + 64])
                else:
                    ldcast(wt[:, :, 0:n], w_in_view(c0, n), [wk], cache=winc[:, :, c0:c0 + n])
                return wt, wk

            def proj_fm(dst, dkey, wt, wk, ncols, bcs, scale):
                for j in range(ncols // 128):
                    for n4 in range(4):
                        pb, pk = nxt(psG, "psG")
                        for kc in range(8):
                            mm(pb[:, :], wt[:, kc, j * 128:(j + 1) * 128], xT[:, kc, n4 * 512:(n4 + 1) * 512],
                               kc == 0, kc == 7, [wk] + xTkeys[n4 * 4:(n4 + 1) * 4], [pk])
                        bc = bcs[j]
                        dve(lambda e, pb=pb, j=j, n4=n4, bc=bc, dst=dst: e.tensor_scalar(
                            out=dst(j)[:, n4 * 512:(n4 + 1) * 512], in0=pb[:, :], scalar1=bcol[:, bc:bc + 1], scalar2=scale,
                            op0=ALU.add, op1=ALU.mult), [pk, "bcol"], [dkey])

            def proj_q(c0, bc0):
                for half in range(2):
                    wt, wk = load_w(c0 + half * 256, 256)
                    proj_fm(lambda j, half=half: QT[:, half * 2 + j, :], "QT", wt, wk, 256,
                            [bc0 + half * 2, bc0 + half * 2 + 1], 0.125)

            def proj_kdup(Kt, kkey, c0, bc):
                wt, wk = load_w(c0, 256, dup64=True)
                for g in range(2):
                    for n4 in range(4):
                        pb, pk = nxt(psG, "psG")
                        for kc in range(8):
                            mm(pb[:, :], wt[:, kc, g * 128:(g + 1) * 128], xT[:, kc, n4 * 512:(n4 + 1) * 512],
                               kc == 0, kc == 7, [wk] + xTkeys[n4 * 4:(n4 + 1) * 4], [pk])
                        for hh in range(2):
                            dve(lambda e, pb=pb, g=g, n4=n4, hh=hh, Kt=Kt: e.tensor_scalar(
                                out=Kt[hh * 64:(hh + 1) * 64, g, hh, n4 * 512:(n4 + 1) * 512], in0=pb[hh * 64:(hh + 1) * 64, :],
                                scalar1=bdup[hh * 64:(hh + 1) * 64, g:g + 1], scalar2=None, op0=ALU.add), [pk, "bdup"], [kkey])

            bdup = sbt([128, 2], F32, "bdup")

            def make_bdup(bc):
                for g in range(2):
                    for hh in range(2):
                        dma("sp", bdup[hh * 64:(hh + 1) * 64, g:g + 1],
                            b_in[bc * 128 + g * 64:bc * 128 + g * 64 + 64].rearrange("(p o) -> p o", o=1),
                            [], ["bdup"], "bd")

            def proj_tok(c0, Vt, vkey, boff, withg):
                n = 152 if withg else 128
                i = wsti[0] % 2
                wsti[0] += 1
                wt = wst[i]
                wk = ("wst", i)
                ldcast(wt[:, :, 0:128], w_in_view(c0, 128), [wk], cache=winc[:, :, c0:c0 + 128])
                if withg:
                    ldcast(wt[:, :, 128:152], w_in_view(2048, 24), [wk], cache=winc[:, :, 2048:2072])
                pool(lambda e: e.memset(Vt[:, :, :, 64:65], 1.0), [], [vkey])
                for t in range(NT):
                    pb, pk = nxt(psG, "psG")
                    for kc in range(8):
                        mm(pb[:, 0:n], xT[:, kc, t * 128:(t + 1) * 128], wt[:, kc, 0:n], kc == 0, kc == 7,
                           [wk, ("xT", t)], [pk])
                    dve(lambda e, pb=pb, t=t: e.tensor_tensor(
                        out=Vt[:, t, :, 0:64], in0=pb[:, 0:128].rearrange("p (g d) -> p g d", g=2),
                        in1=btok[:, boff:boff + 128].rearrange("p (g d) -> p g d", g=2), op=ALU.add), [pk, "btok"], [vkey])
                    if withg:
                        dve(lambda e, pb=pb, t=t: e.tensor_tensor(out=Gt[:, t, :], in0=pb[:, 128:152], in1=btok[:, 384:408],
                                                                   op=ALU.add), [pk, "btok"], ["Gt"])
                if withg:
                    act(lambda e: e.activation(out=Gt[:], in_=Gt[:], func=AF.Sigmoid), ["Gt"], ["Gt"])

            def attn_pv(po, pok, pT, pTk, subs, Vt, vkey, kb, g, ncol, first):
                for s_ in subs:
                    mm(po[:, s_, 0:ncol], pT[:, s_ * 128:(s_ + 1) * 128], Vt[:, kb, g, 0:ncol] if Vt is not None else None,
                       first[0], True, [pTk, vkey], [pok])
                    first[0] = False

            SB = [(psS[0], ("psS", 0)), (psS[1], ("psS", 1)), (psG[0], ("psG", 0)), (psG[1], ("psG", 1))]

            def nxtS():
                i = rot["SB"] % 4
                rot["SB"] += 1
                return SB[i]

            def run_tiles(tiles, lag=3):
                n = len(tiles)
                for i in range(n + lag):
                    if i < n:
                        tiles[i][0]()
                    j = i - lag
                    if j >= 0:
                        tiles[j][1]()

            def mk_group():
                return {"po": None, "pok": None, "first": [True]}

            def grp_po(grp):
                if grp["po"] is None:
                    grp["po"], grp["pok"] = nxt(psO, "psO")
                return grp["po"], grp["pok"]

            proj_q(wcols["qA"], 0)
            make_bdup(4)
            proj_kdup(K1, "K1", wcols["kA"], 4)
            proj_tok(wcols["vA"], V1, "V1", 0, False)
            tilesA = []
            for Q in range(4):
                for h in range(8):
                    grp = mk_group()
                    dls = [dl for dl in range(-1, 4) if 4 * Q + dl >= 0]
                    for dl in dls:
                        st = {}

                        def fs(Q=Q, h=h, dl=dl, st=st):
                            g = h // 4
                            hb = (h % 2) * 64
                            kb = 4 * Q + dl
                            subs = [s_ for s_ in (dl, dl + 1) if 0 <= s_ <= 3]
                            c0, c1 = subs[0] * 128, (subs[-1] + 1) * 128
                            ps, psk = nxtS()
                            mm(ps[:, c0:c1], K1[:, g, h % 2, kb * 128:(kb + 1) * 128],
                               QT[:, h // 2, Q * 512 + c0:Q * 512 + c1], True, False, ["K1", "QT"], [psk])
                            mm(ps[:, c0:c1], identb[:], BTA[:, h, c0 - 128 * dl:c1 - 128 * dl], False, True,
                               ["identb", "BTA"], [psk])
                            pT, pTk = nxt(pTs, "pT")
                            act(lambda e, ps=ps, pT=pT, c0=c0, c1=c1: e.activation(out=pT[:, c0:c1], in_=ps[:, c0:c1], func=AF.Exp),
                                [psk], [pTk])
                            st.update(pT=pT, pTk=pTk, subs=subs, kb=kb, g=g)

                        def fp(Q=Q, h=h, st=st, grp=grp, last=(dl == dls[-1])):
                            po, pok = grp_po(grp)
                            attn_pv(po, pok, st["pT"], st["pTk"], st["subs"], V1, "V1", st["kb"], st["g"], 65, grp["first"])
                            if last:
                                dve(lambda e, po=po, h=h: e.tensor_scalar(out=den[:], in0=po[:, :, 64], scalar1=esink[:, h:h + 1],
                                                                          scalar2=None, op0=ALU.add), [pok, "esink"], ["den"])
                                dve(lambda e: e.reciprocal(rec[:], den[:]), ["den"], ["rec"])
                                dve(lambda e, po=po, h=h, Q=Q: e.tensor_tensor(
                                    out=ya[:, 4 * Q:4 * Q + 4, h * 64:(h + 1) * 64], in0=po[:, :, 0:64],
                                    in1=rec[:, :].unsqueeze(2).to_broadcast([128, 4, 64]), op=ALU.mult), [pok, "rec"], ["ya"])

                        tilesA.append((fs, fp))
            run_tiles(tilesA)

            proj_q(wcols["qB"], 6)
            make_bdup(12)
            proj_kdup(K1, "K1", wcols["kBs"], 12)
            make_bdup(14)
            proj_kdup(K2, "K2", wcols["kBw"], 14)
            proj_tok(wcols["vBs"], V1, "V1", 128, True)
            proj_tok(wcols["vBw"], V2, "V2", 256, False)
            wt, wk = load_w(wcols["kBc"], 256)
            proj_fm(lambda j: KC[:, j, :], "acc", wt, wk, 256, [10, 11], 1.0)
            pool(lambda e: e.memset(VcE[:, :, 64:65], 1.0), [], ["VcE"])
            for g in range(2):
                dma("sp", VcE[0:127, g, 65:97], cd["c_mcs"].ap(), [], ["VcE"], "mcs")
            for kv, w1d in enumerate((w1_k, w1_v)):
                i = wsti[0] % 2
                wsti[0] += 1
                w1t = wst[i]
                wk = ("wst", i)
                w1v = w1t[:, :, :].rearrange("p a b -> p (a b)")
                for lh in range(2):
                    if lh == 1:
                        i = wsti[0] % 2
                        wsti[0] += 1
                        w1t = wst[i]
                        wk = ("wst", i)
                        w1v = w1t[:, :, :].rearrange("p a b -> p (a b)")
                    src = w1d[lh * 1024:(lh + 1) * 1024, :].rearrange("(l d) h -> d l h", d=64)
                    for hh in range(2):
                        ldcast(w1v[hh * 64:(hh + 1) * 64, :].rearrange("p (l h) -> p l h", h=128), src, [wk], p0=hh * 64,
                               cache=w1c[kv, hh * 64:(hh + 1) * 64, lh * 16:(lh + 1) * 16, :])
                    pb2 = psG[0]
                    for l in range(16):
                        mm(pb2[:, 500 + kv:501 + kv], w1v[0:64, l * 128:(l + 1) * 128], posT[0:64, kv, lh * 16 + l:lh * 16 + l + 1],
                           (lh == 0 and l == 0), (lh == 1 and l == 15), [wk, "posT"], [("psG", 0)])
                    for g in range(2):
                        pH, pHk = psO[g], ("psO", g)
                        for l in range(16):
                            la = lh * 16 + l
                            mm(pH[:, 0, 0:127], w1v[g * 64:(g + 1) * 64, l * 128:(l + 1) * 128],
                               KC[g * 64:(g + 1) * 64, kv, la:la + 16 * 126 + 1:16], (la == 0), (la == 31), [wk, "acc"], [pHk])
                dve(lambda e, kv=kv: e.tensor_copy(posb[:, kv:kv + 1], psG[0][:, 500 + kv:501 + kv]), [("psG", 0)], ["posb"])
                for g in range(2):
                    pH, pHk = psO[g], ("psO", g)
                    act(lambda e, pH=pH, kv=kv: e.activation(out=gx[:, 0:127], in_=pH[:, 0, 0:127], func=AF.Identity,
                                                             bias=posb[:, kv:kv + 1], scale=1.0), [pHk, "posb"], ["gx"])
                    dve(lambda e: e.tensor_tensor(out=gt_[:, 0:127], in0=gx[:, 0:127], in1=gx[:, 0:127], op=ALU.mult), ["gx"], ["gt"])
                    dve(lambda e: e.tensor_scalar(out=gt_[:, 0:127], in0=gt_[:, 0:127], scalar1=0.044715, scalar2=1.0,
                                                  op0=ALU.mult, op1=ALU.add), ["gt"], ["gt"])
                    dve(lambda e: e.tensor_tensor(out=gt_[:, 0:127], in0=gt_[:, 0:127], in1=gx[:, 0:127], op=ALU.mult), ["gt", "gx"], ["gt"])
                    act(lambda e: e.activation(out=gs[:, 0:127], in_=gt_[:, 0:127], func=AF.Sigmoid, scale=1.5957691216057308),
                        ["gt"], ["gs"])
                    dve(lambda e: e.tensor_tensor(out=hact[:, 0:127], in0=gx[:, 0:127], in1=gs[:, 0:127], op=ALU.mult),
                        ["gx", "gs"], ["hact"])
                    pb, pk = nxt(psG, "psG")
                    if kv == 0:
                        mm(pb[:, 0:127], w2kd[:], hact[:, 0:127], True, True, ["w2kd", "hact"], [pk])
                        for hh in range(2):
                            dve(lambda e, pb=pb, g=g, hh=hh: e.tensor_copy(KcT[hh * 64:(hh + 1) * 64, g, hh, 0:127], pb[hh * 64:(hh + 1) * 64, 0:127]),
                                [pk], ["KcT"])
                    else:
                        mm(pb[0:127, 0:64], hact[:, 0:127], w2vb[:], True, True, ["w2vb", "hact"], [pk])
                        dve(lambda e, pb=pb, g=g: e.tensor_copy(VcE[0:127, g, 0:64], pb[0:127, 0:64]), [pk], ["VcE"])

            for Q in range(4):
                tilesC = []
                for h in range(8):
                    st = {}

                    def fs(Q=Q, h=h, st=st):
                        g = h // 4
                        hb = (h % 2) * 64
                        ps, psk = nxtS()
                        mm(ps[0:127, :], KcT[:, g, h % 2, 0:127], QT[:, h // 2, Q * 512:(Q + 1) * 512], True, False,
                           ["KcT", "QT"], [psk])
                        mm(ps[0:127, :], identb[0:127, 0:127], maskC[0:127, Q * 512:(Q + 1) * 512], False, True,
                           ["identb", "maskC"], [psk])
                        pT, pTk = nxt(pTs, "pT")
                        act(lambda e, ps=ps, pT=pT: e.activation(out=pT[0:127, :], in_=ps[0:127, :], func=AF.Exp), [psk], [pTk])
                        st.update(pT=pT, pTk=pTk)

                    def fp(Q=Q, h=h, st=st):
                        g = h // 4
                        pT, pTk = st["pT"], st["pTk"]
                        po, pok = nxt(psO, "psO")
                        for s_ in range(4):
                            mm(po[:, s_, 0:97], pT[0:127, s_ * 128:(s_ + 1) * 128], VcE[0:127, g, 0:97], s_ == 0, True,
                               [pTk, "VcE"], [pok])
                        dve(lambda e, po=po: e.tensor_scalar(out=den[:], in0=po[:, :, 64], scalar1=1e-30, scalar2=None, op0=ALU.add),
                            [pok], ["den"])
                        dve(lambda e: e.reciprocal(rec[:], den[:]), ["den"], ["rec"])
                        dve(lambda e, h=h, Q=Q: e.tensor_tensor(out=recg[:], in0=rec[:], in1=Gt[:, 4 * Q:4 * Q + 4, h * 3 + 0], op=ALU.mult),
                            ["rec", "Gt"], ["recg"])
                        dve(lambda e, po=po, h=h: e.tensor_tensor(out=acc[:, :, h * 64:(h + 1) * 64], in0=po[:, :, 0:64],
                                                                  in1=recg[:, :].unsqueeze(2).to_broadcast([128, 4, 64]), op=ALU.mult),
                            [pok, "recg"], ["acc"])
                        if h % 4 == 0:
                            dve(lambda e, po=po, g=g: e.tensor_tensor(out=imp[:, :, g, :], in0=po[:, :, 65:97],
                                                                      in1=rec[:, :].unsqueeze(2).to_broadcast([128, 4, 32]), op=ALU.mult),
                                [pok, "rec"], ["imp"])
                        else:
                            dve(lambda e, po=po, g=g: e.tensor_tensor(out=impm[:, :, g, :], in0=po[:, :, 65:97],
                                                                      in1=rec[:, :].unsqueeze(2).to_broadcast([128, 4, 32]), op=ALU.mult),
                                [pok, "rec"], ["impm"])
                            dve(lambda e, g=g: e.tensor_tensor(out=imp[:, :, g, :], in0=imp[:, :, g, :], in1=impm[:, :, g, :], op=ALU.add),
                                ["imp", "impm"], ["imp"])

                    tilesC.append((fs, fp))
                run_tiles(tilesC)
                dve(lambda e, Q=Q: e.tensor_tensor(out=impm[:], in0=imp[:], in1=nfm[:, 4 * Q:4 * Q + 4, :, :], op=ALU.mult),
                    ["imp", "nfm"], ["impm"])
                dve(lambda e, Q=Q: e.tensor_tensor(out=impm[:], in0=impm[:], in1=addc[:, 4 * Q:4 * Q + 4, :, :], op=ALU.add),
                    ["impm", "addc"], ["impm"])
                for s_ in range(4):
                    for g in range(2):
                        dve(lambda e, s_=s_, g=g: e.max(m8[:, s_ * 2 + g, :], impm[:, s_, g, :]), ["impm"], ["m8"])
                for s_ in range(4):
                    for g in range(2):
                        dve(lambda e, s_=s_, g=g: e.tensor_scalar(out=selb[:, s_, g, :], in0=impm[:, s_, g, :],
                                                                  scalar1=m8[:, s_ * 2 + g, 7:8], scalar2=1.0,
                                                                  op0=ALU.is_ge, op1=ALU.subtract), ["impm", "m8"], ["selb"])
                pbt, pbtk = nxt(psT, "psT")
                for g in range(2):
                    for s_ in range(4):
                        tr(pbt[0:32, g * 4 + s_, :], selb[:, s_, g, :], identb[:], ["selb", "identb"], [pbtk])
                dve(lambda e, pbt=pbt: e.tensor_copy(selT[0:32, :, :], pbt[0:32, :, :].rearrange("p (g s) q -> p g (s q)", g=2)),
                    [pbtk], ["selT"])
                tilesB = []
                for h in range(8):
                    grp = mk_group()
                    dls = [dl for dl in range(-4, 4) if 4 * Q + dl >= 0]
                    for dl in dls:
                        st = {}

                        def fs(Q=Q, h=h, dl=dl, st=st):
                            g = h // 4
                            hb = (h % 2) * 64
                            kb = 4 * Q + dl
                            subs = [s_ for s_ in range(4) if dl <= s_ <= dl + 4]
                            c0, c1 = subs[0] * 128, (subs[-1] + 1) * 128
                            ps, psk = nxtS()
                            mm(ps[:, c0:c1], K2[:, g, h % 2, kb * 128:(kb + 1) * 128],
                               QT[:, h // 2, Q * 512 + c0:Q * 512 + c1], True, False, ["K2", "QT"], [psk])
                            mm(ps[:, c0:c1], identb[:], BTW[:, h, c0 - 128 * dl:c1 - 128 * dl], False, True, ["identb", "BTW"], [psk])
                            pT, pTk = nxt(pTs, "pT")
                            act(lambda e, ps=ps, pT=pT, c0=c0, c1=c1: e.activation(out=pT[:, c0:c1], in_=ps[:, c0:c1], func=AF.Exp),
                                [psk], [pTk])
                            st.update(pT=pT, pTk=pTk, subs=subs, kb=kb, g=g)

                        def fp(Q=Q, h=h, st=st, grp=grp, last=(dl == dls[-1])):
                            po, pok = grp_po(grp)
                            attn_pv(po, pok, st["pT"], st["pTk"], st["subs"], V2, "V2", st["kb"], st["g"], 65, grp["first"])
                            if last:
                                dve(lambda e, po=po: e.reciprocal(rec[:], po[:, :, 64]), [pok], ["rec"])
                                dve(lambda e, h=h, Q=Q: e.tensor_tensor(out=recg[:], in0=rec[:], in1=Gt[:, 4 * Q:4 * Q + 4, h * 3 + 2],
                                                                        op=ALU.mult), ["rec", "Gt"], ["recg"])
                                for s_ in range(4):
                                    dve(lambda e, po=po, h=h, s_=s_: e.scalar_tensor_tensor(
                                        out=acc[:, s_, h * 64:(h + 1) * 64], in0=po[:, s_, 0:64], scalar=recg[:, s_:s_ + 1],
                                        in1=acc[:, s_, h * 64:(h + 1) * 64], op0=ALU.mult, op1=ALU.add), [pok, "recg", "acc"], ["acc"])

                        tilesB.append((fs, fp))
                for h in range(8):
                    grp = mk_group()
                    kbs = list(range(0, 4 * Q + 4))
                    for kb in kbs:
                        st = {}

                        def fs(Q=Q, h=h, kb=kb, st=st):
                            g = h // 4
                            hb = (h % 2) * 64
                            dl = kb - 4 * Q
                            subs = [s_ for s_ in range(4) if s_ >= dl]
                            c0 = subs[0] * 128
                            ps, psk = nxtS()
                            mm(ps[:, c0:512], K1[:, g, h % 2, kb * 128:(kb + 1) * 128],
                               QT[:, h // 2, Q * 512 + c0:(Q + 1) * 512], True, False, ["K1", "QT"], [psk])
                            mm(ps[:, c0:512], exm[:, kb, :], selT[:, g, c0:512], False, dl < -1, ["exm", "selT"], [psk])
                            if dl == -1:
                                mm(ps[:, 0:384], identb[:], BTW[:, h, 128:512], False, False, ["identb", "BTW"], [psk])
                                mm(ps[:, 384:512], identb[:], BTW[:, h, 256:384], False, True, ["identb", "BTW"], [psk])
                            elif dl >= 0:
                                mm(ps[:, c0:512], identb[:], BTW[:, h, c0 - 128 * dl:512 - 128 * dl], False, True,
                                   ["identb", "BTW"], [psk])
                            pT, pTk = nxt(pTs, "pT")
                            if dl < -1:
                                act(lambda e, ps=ps, pT=pT, c0=c0, h=h: e.activation(out=pT[:, c0:512], in_=ps[:, c0:512], func=AF.Exp,
                                                                                     bias=c31bc[:, 8 + h:9 + h], scale=1.0),
                                    [psk, "c31bc"], [pTk])
                            else:
                                act(lambda e, ps=ps, pT=pT, c0=c0: e.activation(out=pT[:, c0:512], in_=ps[:, c0:512], func=AF.Exp),
                                    [psk], [pTk])
                            st.update(pT=pT, pTk=pTk, subs=subs, g=g)

                        def fp(Q=Q, h=h, kb=kb, st=st, grp=grp, last=(kb == kbs[-1])):
                            po, pok = grp_po(grp)
                            attn_pv(po, pok, st["pT"], st["pTk"], st["subs"], V1, "V1", kb, st["g"], 65, grp["first"])
                            if last:
                                dve(lambda e, po=po: e.reciprocal(rec[:], po[:, :, 64]), [pok], ["rec"])
                                dve(lambda e, h=h, Q=Q: e.tensor_tensor(out=recg[:], in0=rec[:], in1=Gt[:, 4 * Q:4 * Q + 4, h * 3 + 1],
                                                                        op=ALU.mult), ["rec", "Gt"], ["recg"])
                                for s_ in range(4):
                                    dve(lambda e, po=po, h=h, s_=s_: e.scalar_tensor_tensor(
                                        out=acc[:, s_, h * 64:(h + 1) * 64], in0=po[:, s_, 0:64], scalar=recg[:, s_:s_ + 1],
                                        in1=acc[:, s_, h * 64:(h + 1) * 64], op0=ALU.mult, op1=ALU.add), [pok, "recg", "acc"], ["acc"])

                        tilesB.append((fs, fp))
                run_tiles(tilesB)
                pool(lambda e, Q=Q: e.tensor_copy(yb[:, 4 * Q:4 * Q + 4, :], acc[:]), ["acc"], ["yb"])
            P.barrier()

        with ExitStack() as es:
            def sbt(shape, dt, name):
                uid[0] += 1
                return es.enter_context(nc.sbuf_tensor("%s_%d" % (name, uid[0]), list(shape), dt))

            ln1g = sbt([128, D], F32, "ln1g"); ldc(ln1g[:], ln1g_d.ap().partition_broadcast(128), "ln1g")
            ln1b = sbt([128, D], F32, "ln1b"); ldc(ln1b[:], ln1b_d.ap().partition_broadcast(128), "ln1b")
            wba = sbt([128, 4, D], BF16, "wba")
            wbb = sbt([128, 4, D], BF16, "wbb")
            wout = sbt([128, 8, D], BF16, "wout")
            wg = sbt([128, 8, 2048], BF16, "wg")
            import itertools
            cvt = itertools.cycle(("act", "dve"))
            for k2 in range(2):
                ldcast(wba[:, 2 * k2:2 * k2 + 2, :], wba_d.ap().rearrange("(kc p) n -> p kc n", p=128)[:, 2 * k2:2 * k2 + 2, :], ["wba"],
                       ceng=next(cvt), cache=wbac[:, 2 * k2:2 * k2 + 2, :])
                ldcast(wbb[:, 2 * k2:2 * k2 + 2, :], wbb_d.ap().rearrange("(kc p) n -> p kc n", p=128)[:, 2 * k2:2 * k2 + 2, :], ["wbb"],
                       ceng=next(cvt), cache=wbbc[:, 2 * k2:2 * k2 + 2, :])
            for k2 in range(4):
                ldcast(wout[:, 2 * k2:2 * k2 + 2, :], wout_d.ap().rearrange("(kc p) n -> p kc n", p=128)[:, 2 * k2:2 * k2 + 2, :], ["wout"],
                       ceng=next(cvt), cache=woutc[:, 2 * k2:2 * k2 + 2, :])
            for q4 in range(4):
                for h4 in range(2):
                    ldcast(wg[:, 4 * h4:4 * h4 + 4, q4 * 512:(q4 + 1) * 512], w_in_view(2072 + q4 * 512, 512)[:, 4 * h4:4 * h4 + 4, :], [("wg", q4)],
                           ceng=next(cvt), cache=winc[:, 4 * h4:4 * h4 + 4, 2072 + q4 * 512:2072 + (q4 + 1) * 512])
            yaT = sbt([128, 4, 512], BF16, "yaT")
            ybT = sbt([128, 4, 512], BF16, "ybT")
            mrg = sbt([128, 8, 512], BF16, "mrg")
            mrg2 = sbt([128, 8, 512], BF16, "mrg2")
            gab = [sbt([128, 512], F32, "ga%d" % i) for i in range(2)]
            gbb = [sbt([128, 512], F32, "gb%d" % i) for i in range(2)]
            xr = [sbt([128, D], F32, "xr%d" % i) for i in range(1)]
            zts = [sbt([128, D], F32, "zt%d" % i) for i in range(2)]
            ht = [sbt([128, D], F32, "ht%d" % i) for i in range(1)]
            hbf = [sbt([128, D], BF16, "hbf%d" % i) for i in range(2)]
            hT = sbt([128, 8, 128], BF16, "hT")
            st6 = sbt([128, 2, 6], F32, "st6")
            mv = sbt([128, 2], F32, "mv")
            rstd = sbt([128, 1], F32, "rstd")
            lg = sbt([128, 32], F32, "lg")
            msk = sbt([128, 32], BF16, "msk")
            slot = sbt([128, 32], F32, "slot")
            tmp32 = sbt([128, 32], F32, "tmp32")
            r8 = sbt([128, 8], F32, "r8")
            nm0 = sbt([128, 1], F32, "nm0")
            ek = sbt([128, 4], F32, "ek")
            esum = sbt([128, 1], F32, "esum")
            destf = sbt([128, 4], F32, "destf")

            mrgs = [mrg, mrg2]

            def stage_mc(T4):
                mrg = mrgs[T4 % 2]
                for (ysrc, yk, yT, yTk) in ((ya, "ya", yaT, "yaT"), (yb, "yb", ybT, "ybT")):
                    for c in range(4):
                        pbt, pbtk = nxt(psT, "psT")
                        for tt in range(4):
                            tr(pbt[:, tt, :], ysrc[:, T4 * 4 + tt, c * 128:(c + 1) * 128], identb[:], [yk, "identb"], [pbtk])
                        act(lambda e, pbt=pbt, c=c, yT=yT: e.copy(yT[:, c, :], pbt[:, 0:4, :].rearrange("p a b -> p (a b)")),
                            [pbtk], [yTk])
                for mc in range(8):
                    ga, gak = nxt(gab, "ga")
                    gb_, gbk = nxt(gbb, "gb")
                    pgA, pgAk = nxt(psS, "psS")
                    for kc in range(8):
                        mm(pgA[:, :], wg[:, kc, mc * 128:(mc + 1) * 128], xT[:, kc, T4 * 512:(T4 + 1) * 512], kc == 0, kc == 7,
                           [("wg", mc // 4)] + xTkeys[T4 * 4:(T4 + 1) * 4], [pgAk])
                    act(lambda e, pgA=pgA, ga=ga, mc=mc: e.activation(out=ga[:], in_=pgA[:, :], func=AF.Sigmoid,
                                                                      bias=bcol[:, 16 + mc:17 + mc], scale=1.0), [pgAk, "bcol"], [gak])
                    pgB, pgBk = nxt(psS, "psS")
                    for kc in range(8):
                        mm(pgB[:, :], wg[:, kc, 1024 + mc * 128:1024 + (mc + 1) * 128], xT[:, kc, T4 * 512:(T4 + 1) * 512],
                           kc == 0, kc == 7, [("wg", 2 + mc // 4)] + xTkeys[T4 * 4:(T4 + 1) * 4], [pgBk])
                    act(lambda e, pgB=pgB, gb_=gb_, mc=mc: e.activation(out=gb_[:], in_=pgB[:, :], func=AF.Sigmoid,
                                                                        bias=bcol[:, 24 + mc:25 + mc], scale=1.0), [pgBk, "bcol"], [gbk])
                    pA, pAk = nxt(psG, "psG")
                    for kc in range(4):
                        mm(pA[:, :], wba[:, kc, mc * 128:(mc + 1) * 128], yaT[:, kc, :], kc == 0, kc == 3, ["wba", "yaT"], [pAk])
                    dve(lambda e, pA=pA, ga=ga: e.tensor_tensor(out=ga[:], in0=pA[:, :], in1=ga[:], op=ALU.mult), [pAk, gak], [gak])
                    pB, pBk = nxt(psG, "psG")
                    for kc in range(4):
                        mm(pB[:, :], wbb[:, kc, mc * 128:(mc + 1) * 128], ybT[:, kc, :], kc == 0, kc == 3, ["wbb", "ybT"], [pBk])
                    dve(lambda e, pB=pB, gb_=gb_: e.tensor_tensor(out=gb_[:], in0=pB[:, :], in1=gb_[:], op=ALU.mult), [pBk, gbk], [gbk])
                    dve(lambda e, mc=mc, ga=ga, gb_=gb_: e.tensor_tensor(out=mrg[:, mc, :], in0=ga[:], in1=gb_[:], op=ALU.add), [gak, gbk], [("mrg", T4 % 2, mc)])
                    yield

            def stage_tt(T4, tt):
                mrg = mrgs[T4 % 2]
                mrgk = [("mrg", T4 % 2, mc) for mc in range(8)]
                tg = sq * NT + T4 * 4 + tt
                r0 = tg * 128
                xrt, xrk = nxt(xr, "xr")
                zt, ztn = nxt(zts, "zt")
                zi = ztn[1]
                dma("sp", xrt[:], x_d[r0:r0 + 128, :], [], [xrk], "xr0")
                for half in range(2):
                    po_, pok_ = nxt(psO, "psO")
                    pov = po_[:, :, :].rearrange("p a b -> p (a b)")
                    for mc in range(8):
                        mm(pov, mrg[:, mc, tt * 128:(tt + 1) * 128], wout[:, mc, half * 512:(half + 1) * 512], mc == 0, mc == 7,
                           mrgk + ["wout"], [pok_])
                    dve(lambda e, pov=pov, xrt=xrt, half=half, zt=zt: e.scalar_tensor_tensor(
                        out=zt[:, half * 512:(half + 1) * 512], in0=xrt[:, half * 512:(half + 1) * 512], scalar=ALPHA, in1=pov,
                        op0=ALU.mult, op1=ALU.add), [pok_, xrk], [("zt", zi, half)])
                    dve(lambda e, half=half, zt=zt: e.bn_stats(st6[:, half, :], zt[:, half * 512:(half + 1) * 512]), [("zt", zi, half)], ["st6"])
                dve(lambda e: e.bn_aggr(mv[:], st6[:]), ["st6"], ["mv"])
                pool(lambda e: e.tensor_scalar(out=rstd[:], in0=mv[:, 1:2], scalar1=LN_EPS, scalar2=None, op0=ALU.add), ["mv"], ["rstd"])
                pool(lambda e: e.tensor_tensor(out=rstd[:], in0=rstd[:], in1=mhalf[:], op=ALU.pow), ["rstd", "mhalf"], ["rstd"])
                htt, htk = nxt(ht, "ht")
                hb_, hbk = nxt(hbf, "hbf")
                dve(lambda e, zt=zt: e.tensor_scalar(out=zt[:], in0=zt[:], scalar1=mv[:, 0:1], scalar2=rstd[:, 0:1],
                                              op0=ALU.subtract, op1=ALU.mult), [("zt", zi, 0), ("zt", zi, 1), "mv", "rstd"], [("zt", zi, 0), ("zt", zi, 1)])
                dve(lambda e, zt=zt: e.tensor_tensor(out=zt[:], in0=zt[:], in1=ln1g[:], op=ALU.mult), [("zt", zi, 0), ("zt", zi, 1), "ln1g"],
                    [("zt", zi, 0), ("zt", zi, 1)])
                dve(lambda e, htt=htt, zt=zt: e.tensor_tensor(out=htt[:], in0=zt[:], in1=ln1b[:], op=ALU.add), [("zt", zi, 0), ("zt", zi, 1), "ln1b"], [htk])
                dma("sp", hres[r0:r0 + 128, :], htt[:], [htk], [("hres", tg)], "hs%d" % (tg % 2))
                if dbg:
                    dma("sp", dbg_h[r0:r0 + 128, :], htt[:], [htk], [], "hd%d" % (tg % 2))
                dve(lambda e, hb_=hb_, zt=zt: e.tensor_tensor(out=hb_[:], in0=zt[:], in1=ln1b[:], op=ALU.add), [("zt", zi, 0), ("zt", zi, 1), "ln1b"], [hbk])
                pend_b.append(lambda: stage_tt_b(tg, hb_, hbk))

            def stage_tt_b(tg, hb_, hbk):
                pbt, pbtk = nxt(psT, "psT")
                for kc in range(8):
                    tr(pbt[:, kc, :], hb_[:, kc * 128:(kc + 1) * 128], identb[:], [hbk, "identb"], [pbtk])
                act(lambda e, pbt=pbt: e.copy(hT[:], pbt[:]), [pbtk], ["hT"])
                pl, plk = nxt(psG, "psG")
                for kc in range(8):
                    mm(pl[:, 0:32], hT[:, kc, :], wr[:, kc, :], kc == 0, kc == 7, ["hT", "wr"], [plk])
                dve(lambda e, pl=pl: e.tensor_tensor(out=lg[:], in0=pl[:, 0:32], in1=brt[:], op=ALU.add), [plk, "brt"], ["lg"])
                dve(lambda e: e.max(r8[:], lg[:]), ["lg"], ["r8"])
                dve(lambda e: e.tensor_scalar(out=msk[:], in0=lg[:], scalar1=r8[:, 3:4], scalar2=None, op0=ALU.is_ge), ["lg", "r8"], ["msk"])
                dve(lambda e, tg=tg: e.tensor_scalar(out=gates_all[:, tg, :], in0=r8[:, 0:4], scalar1=r8[:, 0:1], scalar2=None, op0=ALU.subtract),
                    ["r8"], ["gates_all"])
                pp, ppk = nxt(psG, "psG")
                mm(pp[:, 0:32], uut[:], msk[:], True, True, ["uut", "msk"], [ppk])
                mm(pp[:, 32:64], onesb[:], msk[:], False, True, ["onesb", "msk"], [ppk])
                dve(lambda e, pp=pp: e.tensor_tensor(out=slot[:], in0=pp[:, 0:32], in1=tot[:], op=ALU.add), [ppk, "tot"], ["slot"])
                dve(lambda e, pp=pp: e.tensor_tensor(out=tot[:], in0=pp[:, 32:64], in1=tot[:], op=ALU.add), [ppk, "tot"], ["tot"])
                dve(lambda e: e.scalar_tensor_tensor(out=slot[:], in0=slot[:], scalar=float(CAP - 1), in1=ecap[:], op0=ALU.min, op1=ALU.add),
                    ["slot", "ecap"], ["slot"])
                dve(lambda e: e.memset(destf[:], 0.0), [], ["destf"])
                for k in range(4):
                    dve(lambda e, k=k: e.scalar_tensor_tensor(out=tmp32[:], in0=lg[:], scalar=r8[:, k:k + 1], in1=slot[:],
                                                              op0=ALU.is_equal, op1=ALU.mult, accum_out=destf[:, k:k + 1]),
                        ["lg", "r8", "slot"], ["tmp32", "destf"])
                dve(lambda e: e.tensor_scalar(out=destf[:], in0=destf[:], scalar1=float(NE * CAP - 1), scalar2=None, op0=ALU.min),
                    ["destf"], ["destf"])
                dve(lambda e, tg=tg: e.tensor_copy(dest_all[:, tg, :], destf[:]), ["destf"], [("dest", tg)])
                for k in range(4):
                    P.add("pool", lambda e, tg=tg, k=k, hb_=hb_: e.indirect_dma_start(
                        out=xg.ap(), out_offset=bass.IndirectOffsetOnAxis(ap=dest_all[:, tg, k:k + 1], axis=0),
                        in_=hb_[:], in_offset=None), reads=[("dest", tg), hbk], writes=["xg"], dma="sc%d" % k)

            gen = stage_mc(0)
            for _ in gen:
                pass
            pend_b = []
            for T4 in range(4):
                gen = stage_mc(T4 + 1) if T4 + 1 < 4 else iter(())
                for tt in range(4):
                    stage_tt(T4, tt)
                    if len(pend_b) > 1:
                        pend_b.pop(0)()
                    for _ in range(2):
                        next(gen, None)
                for _ in gen:
                    pass
            while pend_b:
                pend_b.pop(0)()
            P.barrier()
        cache_ready[0] = True

    es_seq.close()
    slabs = []
    o = 0
    while o < CAP:
        n = min(512, CAP - o)
        slabs.append((o, n))
        o += n
    with ExitStack() as es:
        def sbt(shape, dt, name):
            uid[0] += 1
            return es.enter_context(nc.sbuf_tensor("%s_%d" % (name, uid[0]), list(shape), dt))

        wgu = [sbt([128, 8, 2 * D], BF16, "wgu%d" % i) for i in range(2)]
        wdn = [sbt([128, 8, D], BF16, "wdn%d" % i) for i in range(2)]
        bdb = [sbt([128, D], F32, "bdb%d" % i) for i in range(2)]
        xe = [sbt([128, 4, D], BF16, "xe%d" % i) for i in range(2)]
        xeT = [sbt([128, 8, 512], BF16, "xeT%d" % i) for i in range(2)]
        aT = [sbt([128, 8, 512], BF16, "aT%d" % i) for i in range(2)]
        glu = [sbt([128, 512], F32, "glu%d" % i) for i in range(2)]
        sg = [sbt([128, 512], F32, "sg%d" % i) for i in range(2)]
        lin = [sbt([128, 512], F32, "lin%d" % i) for i in range(2)]
        t1 = [sbt([128, 512], F32, "t1%d" % i) for i in range(2)]
        ye = [sbt([128, D], BF16, "ye%d" % i) for i in range(2)]

        def ld_split(dst, src, wkeys, ceng):
            i = stgi[0] % len(stg)
            stgi[0] += 1
            shp = list(dst.shape)
            n = 1
            for d_ in shp[1:]:
                n *= d_
            assert n <= STGN
            sv = stg[i][0:shp[0], 0:n]
            if len(shp) == 3:
                sv = sv.rearrange("p (a b) -> p a b", b=shp[2])
            sk = ("stg", i)
            dma("sp", sv, src, [], [sk], "sg%d" % i)

            def cv():
                if ceng == "act":
                    P.add("act", lambda e: e.copy(dst, sv), reads=[sk], writes=wkeys)
                else:
                    P.add(ceng, lambda e: e.tensor_copy(dst, sv), reads=[sk], writes=wkeys)
            return cv

        def expert_loads(e_):
            i = e_ % 2
            chunks = []
            for kc in range(8):
                for hf in range(2):
                    chunks.append((wgu[i][:, kc, hf * 1024:(hf + 1) * 1024], wgu_d[e_, kc * 128:(kc + 1) * 128, hf * 1024:(hf + 1) * 1024],
                                   [("wgu", i, kc)]))
            for kc in range(8):
                chunks.append((wdn[i][:, kc, :], wd_d[e_, kc * 128:(kc + 1) * 128, :], [("wdn", i, kc)]))
            fl = []
            state = {"cv": None}

            def step(c):
                prev = state["cv"]
                if c < len(chunks):
                    d_, s_, k_ = chunks[c]
                    state["cv"] = ld_split(d_, s_, k_, "act")
                else:
                    state["cv"] = None
                if prev is not None:
                    prev()

            for c in range(len(chunks) + 1):
                fl.append(lambda c=c: step(c))
            fl.append(lambda: dma("sp", bdb[i][:], bd_d[e_, :].partition_broadcast(128), [], [("bdb", i)], "bdb%d" % i))
            return fl

        for f_ in expert_loads(0):
            f_()
        pend_down = []
        work = [(e_, o, n) for e_ in range(NE) for (o, n) in slabs]

        def prep(idx):
            e_, o, n = work[idx]
            ntt = n // 128
            xet, xek = nxt(xe, "xe")
            row0 = e_ * CAP + o
            dma("pool", xet[:, 0:ntt, :], xg[row0:row0 + n, :].rearrange("(t p) d -> p t d", p=128), ["xg"], [xek],
                "xe%d" % (rot["xe"] % 2))
            xT_, xTk = nxt(xeT, "xeT")
            for tt in range(ntt):
                pbt, pbtk = nxt(psT, "psT")
                for kc in range(8):
                    tr(pbt[:, kc, :], xet[:, tt, kc * 128:(kc + 1) * 128], identb[:], [xek, "identb"], [pbtk])
                dve(lambda e, pbt=pbt, tt=tt, xT_=xT_: e.tensor_copy(xT_[:, :, tt * 128:(tt + 1) * 128], pbt[:]), [pbtk], [xTk])
            return dict(xT_=xT_, xTk=xTk, ntt=ntt, row0=row0)

        preps = {0: prep(0)}
        pend = []
        for idx, (e_, o, n) in enumerate(work):
            i = e_ % 2
            if o == 0:
                pend = expert_loads(e_ + 1) if e_ + 1 < NE else []
            wguk = [("wgu", i, kc) for kc in range(8)]
            pr = preps.pop(idx)
            xT_, xTk, ntt, row0 = pr["xT_"], pr["xTk"], pr["ntt"], pr["row0"]
            at, atk = nxt(aT, "aT")
            for fc in range(8):
                if pend:
                    pend.pop(0)()
                pg, pgk = nxt(psS, "psS")
                for kc in range(8):
                    mm(pg[:, 0:n], wgu[i][:, kc, fc * 128:(fc + 1) * 128], xT_[:, kc, 0:n], kc == 0, kc == 7, [wguk[kc], xTk], [pgk])
                plin, plk = nxt(psG, "psG")
                for kc in range(8):
                    mm(plin[:, 0:n], wgu[i][:, kc, D + fc * 128:D + (fc + 1) * 128], xT_[:, kc, 0:n], kc == 0, kc == 7,
                       [wguk[kc], xTk], [plk])
                if fc == 1 and pend_down:
                    pend_down.pop(0)()
                if fc == 4 and idx + 1 < len(work):
                    preps[idx + 1] = prep(idx + 1)
                g_, gk = nxt(glu, "glu")
                s__, sk = nxt(sg, "sg")
                l_, lk = nxt(lin, "lin")
                t_, tk = nxt(t1, "t1")
                bg = bguT[:, e_ * 16 + fc:e_ * 16 + fc + 1]
                bl = bguT[:, e_ * 16 + 8 + fc:e_ * 16 + 8 + fc + 1]
                dve(lambda e, pg=pg, g_=g_, bg=bg, n=n: e.tensor_scalar(out=g_[:, 0:n], in0=pg[:, 0:n], scalar1=bg, scalar2=7.0,
                                                                       op0=ALU.add, op1=ALU.min), [pgk, "bguT"], [gk])
                act(lambda e, g_=g_, s__=s__, n=n: e.activation(out=s__[:, 0:n], in_=g_[:, 0:n], func=AF.Sigmoid, scale=1.702), [gk], [sk])
                act(lambda e, plin=plin, l_=l_, bl=bl, n=n: e.activation(out=l_[:, 0:n], in_=plin[:, 0:n], func=AF.Identity, bias=bl, scale=1.0),
                    [plk, "bguT"], [lk])
                dve(lambda e, l_=l_, n=n: e.tensor_scalar(out=l_[:, 0:n], in0=l_[:, 0:n], scalar1=7.0, scalar2=-7.0,
                                                         op0=ALU.min, op1=ALU.max), [lk], [lk])
                dve(lambda e, g_=g_, s__=s__, t_=t_, n=n: e.tensor_tensor(out=t_[:, 0:n], in0=g_[:, 0:n], in1=s__[:, 0:n], op=ALU.mult),
                    [gk, sk], [tk])
                dve(lambda e, t_=t_, l_=l_, at=at, fc=fc, n=n: e.scalar_tensor_tensor(out=at[:, fc, 0:n], in0=l_[:, 0:n], scalar=1.0, in1=t_[:, 0:n],
                                                                                     op0=ALU.add, op1=ALU.mult), [tk, lk], [(atk, fc)])

            def down(at=at, atk=atk, ntt=ntt, row0=row0, i=i):
                atks = [(atk, fc) for fc in range(8)]
                for tt in range(ntt):
                    yt, ytk = nxt(ye, "ye")
                    for half in range(2):
                        po_, pok_ = nxt(psO, "psO")
                        pov = po_[:, :, :].rearrange("p a b -> p (a b)")
                        for fc in range(8):
                            mm(pov, at[:, fc, tt * 128:(tt + 1) * 128], wdn[i][:, fc, half * 512:(half + 1) * 512], fc == 0, fc == 7,
                               atks + [("wdn", i, fc)], [pok_])
                        dve(lambda e, pov=pov, yt=yt, half=half, i=i: e.tensor_tensor(
                            out=yt[:, half * 512:(half + 1) * 512], in0=pov, in1=bdb[i][:, half * 512:(half + 1) * 512], op=ALU.add),
                            [pok_, ("bdb", i)], [(ytk, half)])
                    r0 = row0 + tt * 128
                    dma("pool", yg[r0:r0 + 128, :], yt[:], [(ytk, 0), (ytk, 1)], ["yg"], "ys%d" % (rot["ye"] % 2))

            pend_down.append(down)
            if o + n >= CAP:
                while pend:
                    pend.pop(0)()
        while pend_down:
            pend_down.pop(0)()
        P.barrier()

    with ExitStack() as es:
        def sbt(shape, dt, name):
            uid[0] += 1
            return es.enter_context(nc.sbuf_tensor("%s_%d" % (name, uid[0]), list(shape), dt))

        ln2g = sbt([128, D], F32, "ln2g")
        ln2b = sbt([128, D], F32, "ln2b")
        dma("sp", ln2g[:], ln2g_d.ap().partition_broadcast(128), [], ["ln2g"], "c0")
        dma("sp", ln2b[:], ln2b_d.ap().partition_broadcast(128), [], ["ln2b"], "c1")
        gsum = sbt([128, NTT], F32, "gsum")
        act(lambda e: e.activation(out=gates_all[:], in_=gates_all[:], func=AF.Exp), ["gates_all"], ["gates_all"])
        dve(lambda e: e.tensor_reduce(out=gsum[:], in_=gates_all[:], axis=mybir.AxisListType.X, op=ALU.add), ["gates_all"], ["gsum"])
        dve(lambda e: e.reciprocal(gsum[:], gsum[:]), ["gsum"], ["gsum"])
        dve(lambda e: e.tensor_tensor(out=gates_all[:], in0=gates_all[:], in1=gsum[:, :].unsqueeze(2).to_broadcast([128, NTT, 4]), op=ALU.mult),
            ["gates_all", "gsum"], ["gates_all"])
        NBC = 3
        hr = [sbt([128, D], F32, "hr%d" % i) for i in range(NBC)]
        yk4 = [[sbt([128, D], BF16, "yk%d_%d" % (i, k)) for k in range(4)] for i in range(NBC)]
        zz = [sbt([128, D], F32, "zz%d" % i) for i in range(NBC)]
        oo = [sbt([128, D], F32, "oo%d" % i) for i in range(2)]
        st6b = sbt([128, 2, 6], F32, "st6b")
        mvb = sbt([128, 2], F32, "mvb")
        rstdb = sbt([128, 1], F32, "rstdb")
        nmrb = sbt([128, 1], F32, "nmrb")

        def c_loads(tg):
            i = tg % NBC
            r0 = tg * 128
            dma("sp", hr[i][:], hres[r0:r0 + 128, :], [("hres", tg)], [("hr", i)], "hr%d" % i)
            for k in range(4):
                P.add("pool", lambda e, tg=tg, k=k, i=i: e.indirect_dma_start(
                    out=yk4[i][k][:], out_offset=None, in_=yg.ap(),
                    in_offset=bass.IndirectOffsetOnAxis(ap=dest_all[:, tg, k:k + 1], axis=0)),
                    reads=["yg"], writes=[("yk", i, k)], dma="gk%d%d" % (i, k))

        dgs = [[sbt([128, 128], BF16, "dg%d_%d" % (i, k)) for k in range(4)] for i in range(2)]

        st6c = [sbt([128, 2, 6], F32, "st6c%d" % i) for i in range(2)]
        mvc = [sbt([128, 2], F32, "mvc%d" % i) for i in range(2)]
        rstdc = [sbt([128, 1], F32, "rstdc%d" % i) for i in range(2)]
        nmrc = [sbt([128, 1], F32, "nmrc%d" % i) for i in range(2)]

        def c_compute1(tg):
            i = tg % NBC
            j = tg % 2
            z = zz[i]
            zk = ("zz", i)
            for k in range(4):
                act(lambda e, j=j, k=k, tg=tg: e.activation(out=dgs[j][k][:], in_=identb[:], func=AF.Copy, scale=gates_all[:, tg, k:k + 1]),
                    ["identb", "gates_all"], [("dg", j, k)])
            for half in range(2):
                po_, pok_ = nxt(psS, "psS")
                for k in range(4):
                    mm(po_[:, :], dgs[j][k][:], yk4[i][k][:, half * 512:(half + 1) * 512], k == 0, k == 3, [("dg", j, k), ("yk", i, k)], [pok_])
                dve(lambda e, z=z, i=i, half=half, po_=po_: e.scalar_tensor_tensor(
                    out=z[:, half * 512:(half + 1) * 512], in0=hr[i][:, half * 512:(half + 1) * 512], scalar=ALPHA, in1=po_[:, :],
                    op0=ALU.mult, op1=ALU.add), [pok_, ("hr", i)], [(zk, half)])
                dve(lambda e, z=z, half=half, j=j: e.bn_stats(st6c[j][:, half, :], z[:, half * 512:(half + 1) * 512]), [(zk, half)], [("st6c", j)])
            dve(lambda e, j=j: e.bn_aggr(mvc[j][:], st6c[j][:]), [("st6c", j)], [("mvc", j)])
            act(lambda e, j=j: e.activation(out=rstdc[j][:], in_=mvc[j][:, 1:2], func=AF.Ln, bias=epsb[:, 0:1], scale=1.0), [("mvc", j), "epsb"], [("rstdc", j)])
            act(lambda e, j=j: e.activation(out=rstdc[j][:], in_=rstdc[j][:], func=AF.Exp, scale=-0.5), [("rstdc", j)], [("rstdc", j)])
            dve(lambda e, j=j: e.scalar_tensor_tensor(out=nmrc[j][:], in0=mvc[j][:, 0:1], scalar=-1.0, in1=rstdc[j][:], op0=ALU.mult, op1=ALU.mult),
                [("mvc", j), ("rstdc", j)], [("nmrc", j)])

        def c_compute2(tg):
            i = tg % NBC
            j = tg % 2
            r0 = tg * 128
            z = zz[i]
            zk = ("zz", i)
            zks = [(zk, 0), (zk, 1)]
            act(lambda e, z=z, j=j: e.activation(out=z[:], in_=z[:], func=AF.Identity, bias=nmrc[j][:, 0:1], scale=rstdc[j][:, 0:1]),
                zks + [("nmrc", j), ("rstdc", j)], zks)
            dve(lambda e, z=z: e.tensor_tensor(out=z[:], in0=z[:], in1=ln2g[:], op=ALU.mult), zks + ["ln2g"], zks)
            dve(lambda e, z=z, j=j: e.tensor_tensor(out=oo[j][:], in0=z[:], in1=ln2b[:], op=ALU.add), zks + ["ln2b"], [("oo", j)])
            dma("sp", out_d[r0:r0 + 128, :], oo[j][:], [("oo", j)], [], "os%d" % j)

        c_loads(0)
        if NTT > 1:
            c_loads(1)
        for tg in range(NTT):
            if tg + 2 < NTT:
                c_loads(tg + 2)
            c_compute1(tg)
            if tg >= 1:
                c_compute2(tg - 1)
        c_compute2(NTT - 1)
    P.emit()
    return nc


def _prep_inputs(inputs, NSEQ, CAP, ncores):
    f32 = lambda a: np.ascontiguousarray(np.asarray(a, dtype=np.float32))
    x = f32(inputs["x"])
    shared = {}
    for k in ("w_in", "b_in", "attn_sinks", "cmp_pos_k", "cmp_w1_k", "cmp_w2_k", "cmp_pos_v", "cmp_w1_v", "cmp_w2_v",
              "w_branch_a", "w_branch_b", "w_out", "ln1_g", "ln1_b", "w_router", "b_router", "w_gate_up", "b_gate_up",
              "w_down", "b_down", "ln2_g", "ln2_b"):
        shared[k] = f32(inputs[k])[0]
    shared["rel_bias"] = f32(inputs["rel_bias"])
    shared.update(make_consts(CAP))
    in_maps = []
    for c in range(ncores):
        m = dict(shared)
        m["x"] = np.ascontiguousarray(x[c * NSEQ:(c + 1) * NSEQ].reshape(NSEQ * S, D))
        in_maps.append(m)
    return in_maps


def kernel(**inputs):
    NSEQ, CAP, ncores = 4, 1280, 8
    nc = build(NSEQ, CAP)
    in_maps = _prep_inputs(inputs, NSEQ, CAP, ncores)
    res = run_bass_kernel_spmd(nc, in_maps, core_ids=list(range(ncores)))
    out = np.stack([np.asarray(r["out"]).reshape(NSEQ, S, D) for r in res.results], axis=0)
    return out.reshape(ncores * NSEQ, S, D).astype(np.float32)
```
